# Optimizing a Trainium2 kernel written in Bass

```python
import math
import jax, jax.numpy as jnp
from jax import lax
import numpy as np

D_MODEL = 1024
BATCH = 8
SEQ = 2048
DEPTH = 2

CHUNK = 64
N_MIXERS = 2
N_HEADS = 16
HEAD_DIM = D_MODEL // N_HEADS
Q_BLOCK = 128
CONV_WIDTH = 31
D_FF = 2816
N_EXPERTS = 8
TOP_K = 2
D_FF_EXPERT = 1408
D_PLE = 256
LN_EPS = 1e-5
ALPHA = (2.0 * DEPTH) ** 0.25
BETA = (8.0 * DEPTH) ** -0.25
N_CONV = (DEPTH + 1) // 2
N_ATTN = DEPTH // 2

kernel_name = "hybrid_conv_stickbreaking_moe_deepnorm"


def layer_norm(x, g, b):
    xf = x.astype(jnp.float32)
    mu = jnp.mean(xf, axis=-1, keepdims=True)
    var = jnp.mean(jnp.square(xf - mu), axis=-1, keepdims=True)
    y = (xf - mu) * lax.rsqrt(var + LN_EPS)
    return (y * g + b).astype(x.dtype)


def conformer_conv(x, w_pw1, b_pw1, w_dw, b_dw, ln_g, ln_b, w_pw2, b_pw2):
    h = x @ w_pw1 + b_pw1
    a, g = jnp.split(h, 2, axis=-1)
    h = a * jax.nn.sigmoid(g)
    h = lax.conv_general_dilated(
        h, w_dw[:, None, :].astype(h.dtype),
        window_strides=(1,),
        padding=((CONV_WIDTH - 1, 0),),
        dimension_numbers=("NWC", "WIO", "NWC"),
        feature_group_count=D_MODEL) + b_dw
    h = jax.nn.silu(layer_norm(h, ln_g, ln_b))
    return h @ w_pw2 + b_pw2


def stick_breaking_attention(x, w_qkv, w_o):
    B, S, _ = x.shape
    qkv = (x @ w_qkv).reshape(B, S, 3, N_HEADS, HEAD_DIM)
    q, k, v = qkv[:, :, 0], qkv[:, :, 1], qkv[:, :, 2]
    scale = HEAD_DIM ** -0.5
    outs = []
    for qb in range(S // Q_BLOCK):
        q0 = qb * Q_BLOCK
        q1 = q0 + Q_BLOCK
        qi = q[:, q0:q1]
        kj = k[:, :q1]
        vj = v[:, :q1]
        z = jnp.einsum("bthd,bshd->bhts", qi, kj).astype(jnp.float32) * scale
        t_pos = q0 + jnp.arange(Q_BLOCK)[:, None]
        s_pos = jnp.arange(q1)[None, :]
        causal = s_pos < t_pos
        log_beta = jax.nn.log_sigmoid(z)
        log_1m_beta = jnp.where(causal, jax.nn.log_sigmoid(-z), 0.0)
        between = lax.cumsum(log_1m_beta, axis=log_1m_beta.ndim - 1, reverse=True) - log_1m_beta
        w = jnp.where(causal, jnp.exp(log_beta + between), 0.0)
        outs.append(jnp.einsum("bhts,bshd->bthd", w.astype(vj.dtype), vj))
    o = jnp.concatenate(outs, axis=1).reshape(B, S, D_MODEL)
    return o @ w_o


def swiglu(x, w_gate, w_up, w_down):
    return (jax.nn.silu(x @ w_gate) * (x @ w_up)) @ w_down


def moe_swiglu(x, w_router, b_router, w_gate, w_up, w_down):
    B, S, D = x.shape
    xt = x.reshape(B * S, D)
    logits = (xt @ w_router + b_router).astype(jnp.float32)
    top_val, top_idx = lax.top_k(logits, TOP_K)
    top_w = jax.nn.softmax(top_val, axis=-1)
    gates = jnp.sum(jax.nn.one_hot(top_idx, N_EXPERTS, dtype=jnp.float32)
                    * top_w[..., None], axis=1)
    y = jnp.zeros_like(xt)
    for e in range(N_EXPERTS):
        y = y + gates[:, e:e + 1].astype(xt.dtype) * swiglu(xt, w_gate[e], w_up[e], w_down[e])
    return y.reshape(B, S, D)


def setup_inputs(seed: int = 0) -> dict:
    key = jax.random.key(seed)
    ks = jax.random.split(key, 32)
    f32 = jnp.float32
    D = D_MODEL

    def nrm(k, shape, scale):
        return jax.random.normal(k, shape, f32) * scale

    return {
        "x": nrm(ks[0], (BATCH, SEQ, D), 1.0),
        "p": nrm(ks[1], (DEPTH, BATCH, SEQ, D_PLE), 1.0),
        "conv_w_pw1": nrm(ks[2], (N_CONV, D, 2 * D), D ** -0.5),
        "conv_b_pw1": nrm(ks[3], (N_CONV, 2 * D), 0.02),
        "conv_w_dw": nrm(ks[4], (N_CONV, CONV_WIDTH, D), CONV_WIDTH ** -0.5),
        "conv_b_dw": nrm(ks[5], (N_CONV, D), 0.02),
        "conv_ln_g": 1.0 + nrm(ks[6], (N_CONV, D), 0.02),
        "conv_ln_b": nrm(ks[7], (N_CONV, D), 0.02),
        "conv_w_pw2": nrm(ks[8], (N_CONV, D, D), BETA * D ** -0.5),
        "conv_b_pw2": nrm(ks[9], (N_CONV, D), 0.02),
        "attn_w_qkv": nrm(ks[10], (N_ATTN, D, 3 * D), D ** -0.5),
        "attn_w_o": nrm(ks[11], (N_ATTN, D, D), BETA * D ** -0.5),
        "ffn_w_gate": nrm(ks[12], (N_CONV, D, D_FF), D ** -0.5),
        "ffn_w_up": nrm(ks[13], (N_CONV, D, D_FF), D ** -0.5),
        "ffn_w_down": nrm(ks[14], (N_CONV, D_FF, D), BETA * D_FF ** -0.5),
        "moe_w_router": nrm(ks[15], (N_ATTN, D, N_EXPERTS), D ** -0.5),
        "moe_b_router": nrm(ks[16], (N_ATTN, N_EXPERTS), 0.01),
        "moe_w_gate": nrm(ks[17], (N_ATTN, N_EXPERTS, D, D_FF_EXPERT), D ** -0.5),
        "moe_w_up": nrm(ks[18], (N_ATTN, N_EXPERTS, D, D_FF_EXPERT), D ** -0.5),
        "moe_w_down": nrm(ks[19], (N_ATTN, N_EXPERTS, D_FF_EXPERT, D), BETA * D_FF_EXPERT ** -0.5),
        "ln_mix_g": 1.0 + nrm(ks[20], (DEPTH, D), 0.02),
        "ln_mix_b": nrm(ks[21], (DEPTH, D), 0.02),
        "ln_ffn_g": 1.0 + nrm(ks[22], (DEPTH, D), 0.02),
        "ln_ffn_b": nrm(ks[23], (DEPTH, D), 0.02),
        "ple_w_proj": nrm(ks[24], (DEPTH, D_PLE, D), D_PLE ** -0.5),
        "ple_w_gate": nrm(ks[25], (DEPTH, D, D), D ** -0.5),
        "ple_b_gate": nrm(ks[26], (DEPTH, D), 0.02),
    }


def reference(x, p, conv_w_pw1, conv_b_pw1, conv_w_dw, conv_b_dw, conv_ln_g, conv_ln_b,
              conv_w_pw2, conv_b_pw2, attn_w_qkv, attn_w_o, ffn_w_gate, ffn_w_up, ffn_w_down,
              moe_w_router, moe_b_router, moe_w_gate, moe_w_up, moe_w_down,
              ln_mix_g, ln_mix_b, ln_ffn_g, ln_ffn_b, ple_w_proj, ple_w_gate, ple_b_gate):
    h = x
    for i in range(DEPTH):
        j = i // N_MIXERS
        if i % N_MIXERS == 0:
            mix = conformer_conv(h, conv_w_pw1[j], conv_b_pw1[j], conv_w_dw[j], conv_b_dw[j],
                                 conv_ln_g[j], conv_ln_b[j], conv_w_pw2[j], conv_b_pw2[j])
        else:
            mix = stick_breaking_attention(h, attn_w_qkv[j], attn_w_o[j])
        h = layer_norm(ALPHA * h + mix, ln_mix_g[i], ln_mix_b[i])
        if i % 2 == 0:
            ff = swiglu(h, ffn_w_gate[j], ffn_w_up[j], ffn_w_down[j])
        else:
            ff = moe_swiglu(h, moe_w_router[j], moe_b_router[j], moe_w_gate[j],
                            moe_w_up[j], moe_w_down[j])
        h = layer_norm(ALPHA * h + ff, ln_ffn_g[i], ln_ffn_b[i])
        gate = jax.nn.sigmoid(h @ ple_w_gate[i] + ple_b_gate[i])
        h = h + gate * (p[i] @ ple_w_proj[i])
    return h
```

```python
import numpy as np
from contextlib import ExitStack
import concourse.bass as bass
import concourse.mybir as mybir
from concourse.bass_utils import run_bass_kernel_spmd

F32 = mybir.dt.float32
BF16 = mybir.dt.bfloat16
AF = mybir.ActivationFunctionType
ALU = mybir.AluOpType

EPOCH = 2048
T = 2048
D = 1024
KC = 8
TW = 512
NTC = T // TW
DFF = 2816
NFC = DFF // 128
NE = 8
DFE = 1408
NFE = DFE // 128
DPLE = 256
CW = 31
ALPHA = 4.0 ** 0.25
EPS = 1e-5
NSLOT = 4


class Prog:
    ENG = ('pe', 'act', 'dve', 'pool', 'sp')

    def __init__(self, nc, stack):
        self.nc = nc
        self.stack = stack
        self.stream = {e: [] for e in self.ENG}
        self.last_w = {}
        self.readers = {}
        self.dma_cnt = {}
        self.dma_sem = {}
        self.last_tok = {}

    def _deps(self, reads, writes):
        deps = set()
        for c in reads:
            t = self.last_w.get(c)
            if t is not None:
                deps.add(t)
        for c in writes:
            t = self.last_w.get(c)
            if t is not None:
                deps.add(t)
            r = self.readers.get(c)
            if r:
                deps.update(r.values())
        out = set()
        for t in deps:
            if t[0] == 'd':
                out.add(('d', t[1], 16 * self.dma_cnt[t[1]]))
            else:
                out.add(t)
        return out

    def _commit(self, tok, key, reads, writes):
        for c in reads:
            self.readers.setdefault(c, {})[key] = tok
        for c in writes:
            self.last_w[c] = tok
            self.readers[c] = {}

    def op(self, eng, fn, reads=(), writes=(), inc=True):
        deps = self._deps(reads, writes)
        pos = len(self.stream[eng])
        tok = ('c', eng, pos)
        self.stream[eng].append(dict(kind='op', fn=fn, deps=deps, inc=inc))
        self._commit(tok, eng, reads, writes)
        self.last_tok[eng] = tok
        return tok

    def dma(self, eng, out, in_, reads=(), writes=(), slot=None, **kw):
        deps = self._deps(reads, writes)
        n = self.dma_cnt.get(slot, 0) + 1
        self.dma_cnt[slot] = n
        tok = ('d', slot, 16 * n)
        self.stream[eng].append(dict(kind='dma', out=out, in_=in_, deps=deps, slot=slot, kw=kw))
        self._commit(tok, ('d', slot, n), reads, writes)
        self.last_tok[('d', slot)] = tok
        return tok

    def wait_all(self, eng, toks):
        self.stream[eng].append(dict(kind='wait', deps=set(toks)))

    def barrier(self):
        toks = set(self.last_tok.values())
        for e in self.ENG:
            self.stream[e].append(dict(kind='wait', deps=set(toks)))
        self.last_w = {}
        self.readers = {}

    def emit(self):
        nc = self.nc
        gidx = {}
        for e in self.ENG:
            s = self.stream[e]
            g = 0
            idx = [None] * len(s)
            for i, r in enumerate(s):
                if r['kind'] == 'op' and r['inc']:
                    g += 1
                    r['g'] = g
            nxt = None
            for i in range(len(s) - 1, -1, -1):
                r = s[i]
                if r['kind'] == 'op' and r['inc']:
                    nxt = r['g']
                idx[i] = nxt
            gidx[e] = idx
        sems = {}
        for e in self.ENG:
            tot = max([r.get('g', 0) for r in self.stream[e]] + [0])
            for ep in range((tot + EPOCH - 1) // EPOCH):
                sems[(e, ep)] = self.stack.enter_context(nc.semaphore(f"s_{e}_{ep}"))
        for slot in self.dma_cnt:
            self.dma_sem[slot] = self.stack.enter_context(nc.semaphore(f"d_{slot}"))
        block = self.stack.enter_context(nc.Block())
        engobj = {'pe': block.tensor, 'act': block.scalar, 'dve': block.vector,
                  'pool': block.gpsimd, 'sp': block.sync}

        def make(e):
            def body(eng):
                waited = {}
                for pos, r in enumerate(self.stream[e]):
                    need = {}
                    for t in r['deps']:
                        if t[0] == 'c':
                            _, de, dp = t
                            if de == e and (e == 'pe' or dp >= pos):
                                continue
                            g = gidx[de][dp]
                            assert g is not None, (e, pos, t)
                            k = ('c', de)
                            need[k] = max(need.get(k, 0), g)
                        else:
                            _, slot, val = t
                            k = ('d', slot)
                            need[k] = max(need.get(k, 0), val)
                    for k, v in need.items():
                        if waited.get(k, 0) >= v:
                            continue
                        waited[k] = v
                        if k[0] == 'c':
                            ep = (v - 1) // EPOCH
                            eng.wait_ge(sems[(k[1], ep)], (v - 1) % EPOCH + 1)
                        else:
                            eng.wait_ge(self.dma_sem[k[1]], v)
                    if r['kind'] == 'op':
                        ins = r['fn'](eng)
                        if r['inc']:
                            g = r['g']
                            ins.then_inc(sems[(e, (g - 1) // EPOCH)], 1)
                    elif r['kind'] == 'dma':
                        eng.dma_start(out=r['out'], in_=r['in_'], **r['kw']).then_inc(
                            self.dma_sem[r['slot']], 16)
            return body

        for e in self.ENG:
            if self.stream[e]:
                engobj[e](make(e))


def _col(v):
    v = np.asarray(v, np.float32)
    return v.reshape(-1, 128).T


PV = {}


def _pv_layout():
    off = 0
    for name, w in [('b_pw1', 16), ('b_dw', 8), ('cln_g', 8), ('cln_b', 8), ('b_pw2', 8),
                    ('wdw', 8 * CW),
                    ('lnm_g0', 8), ('lnm_b0', 8), ('lnf_g0', 8), ('lnf_b0', 8), ('ple_b0', 8),
                    ('lnm_g1', 8), ('lnm_b1', 8), ('lnf_g1', 8), ('lnf_b1', 8), ('ple_b1', 8)]:
        PV[name] = (off, w)
        off += w
    return off


NPV = _pv_layout()


def pack_pvec(inp):
    pv = np.zeros((128, NPV), np.float32)

    def put(name, arr):
        o, w = PV[name]
        assert arr.shape == (128, w), (name, arr.shape)
        pv[:, o:o + w] = arr
    put('b_pw1', _col(inp['conv_b_pw1'][0]))
    put('b_dw', _col(inp['conv_b_dw'][0]))
    put('cln_g', _col(inp['conv_ln_g'][0]))
    put('cln_b', _col(inp['conv_ln_b'][0]))
    put('b_pw2', _col(inp['conv_b_pw2'][0]))
    wd = np.asarray(inp['conv_w_dw'][0], np.float32)
    wdw = wd.reshape(CW, 8, 128).transpose(2, 1, 0).reshape(128, 8 * CW)
    put('wdw', wdw)
    for i in range(2):
        put(f'lnm_g{i}', _col(inp['ln_mix_g'][i]))
        put(f'lnm_b{i}', _col(inp['ln_mix_b'][i]))
        put(f'lnf_g{i}', _col(inp['ln_ffn_g'][i]))
        put(f'lnf_b{i}', _col(inp['ln_ffn_b'][i]))
        put(f'ple_b{i}', _col(inp['ple_b_gate'][i]))
    return pv


class Builder:
    def __init__(self, layers=(0, 1), dense_moe=True, stop=None):
        self.layers = layers
        self.stop = stop
        nc = self.nc = bass.Bass("TRN2", target_bir_lowering=False)
        dt = lambda name, shape, kind="ExternalInput": nc.dram_tensor(name, shape, F32, kind=kind).ap()
        self.xT = dt("xT", [D, T])
        self.pT = dt("pT", [2, DPLE, T])
        self.pvec_d = dt("pvec", [128, NPV])
        self.outT = dt("outT", [D, T], kind="ExternalOutput")
        if 0 in layers:
            self.w_pw1 = dt("w_pw1", [D, 2 * D])
            self.w_pw2 = dt("w_pw2", [D, D])
            self.w_fg = dt("w_fg", [D, DFF])
            self.w_fu = dt("w_fu", [D, DFF])
            self.w_fd = dt("w_fd", [DFF, D])
        if 1 in layers:
            self.w_qkv = dt("w_qkv", [D, 3 * D])
            self.w_o = dt("w_o", [D, D])
            self.w_r = dt("w_r", [D, NE])
            self.b_r = dt("b_r", [128, NE])
            self.w_mg = dt("w_mg", [NE, D, DFE])
            self.w_mu = dt("w_mu", [NE, D, DFE])
            self.w_md = dt("w_md", [NE, DFE, D])
        self.w_pg = dt("w_pg", [2, D, D])
        self.w_pp = dt("w_pp", [2, DPLE, D])
        self.cnt = {}
        self.out_toks = []
        self.stored = set()
        self.slot_live = {}
        self.slot_pool = list(range(NSLOT))

    def alt(self, key, n=2):
        v = self.cnt.get(key, 0)
        self.cnt[key] = v + 1
        return v % n

    def A(self, out, in_, func, reads, writes, scale=None, bias=None):
        kw = {}
        if scale is not None:
            kw['scale'] = scale
        if bias is not None:
            kw['bias'] = bias
        self.p.op('act', lambda e: e.activation(out=out, in_=in_, func=func, **kw), reads, writes)

    def TT(self, eng, out, in0, in1, op, reads, writes):
        self.p.op(eng, lambda e: e.tensor_tensor(out=out, in0=in0, in1=in1, op=op), reads, writes)

    def STT(self, out, in0, scalar, in1, op0, op1, reads, writes):
        self.p.op('dve', lambda e: e.scalar_tensor_tensor(out=out, in0=in0, scalar=scalar, in1=in1,
                                                          op0=op0, op1=op1), reads, writes)

    def MM(self, out, lhsT, rhs, start, stop, reads, writes, inc, skip=False):
        self.p.op('pe', lambda e: e.matmul(out, lhsT=lhsT, rhs=rhs, start=start, stop=stop, skip_group_check=skip),
                  reads, writes, inc=inc)

    def pvc(self, name, c):
        o, w = PV[name]
        return self.pv[:, o + c:o + c + 1]

    def wload(self, segs):
        pool_ = self.slot_pool
        s = pool_[self.alt(('wslot', len(pool_)), len(pool_))]
        assert not self.slot_live.get(s, False), f"weight slot {s} overwritten while its handle is live"
        self.slot_live[s] = True
        off = 0
        offs = []
        for i, (ap, a, b) in enumerate(segs):
            dst = self.WS[s][:, off:off + a * b].rearrange("p (a b) -> p a b", b=b)
            self.p.dma('pool', dst, ap, writes=[('WS', s, i)], slot=f"ws{s}")
            offs.append(off)
            off += a * b
        assert off <= 4096
        return s, offs, [('WS', s, i) for i in range(len(segs))]

    def _release(self, h):
        if h is None:
            return
        if isinstance(h, tuple) and len(h) == 3 and isinstance(h[0], int):
            self.slot_live[h[0]] = False
        elif isinstance(h, tuple):
            for x in h:
                self._release(x)

    def wview(self, s, off, a, b):
        return self.WS[s][:, off:off + a * b].rearrange("p (a b) -> p a b", b=b)

    def pipeline(self, stages, lookahead=NSLOT - 1):
        handles = {}
        n = len(stages)
        for i in range(n + lookahead):
            if i < n:
                handles[i] = stages[i][0]()
            j = i - lookahead
            if j >= 0:
                h = handles.pop(j)
                stages[j][1](h)
                self._release(h)

    def ln_stat(self, X, Xcells, c, banks=(6, 7)):
        p = self.p
        bm, bq = banks
        M, Q = self.PS[bm], self.PS[bq]
        b = self.alt(('lnrot', len(self.R16)), len(self.R16))
        r16, rq16 = self.R16[b], self.RQ16[b]
        p.op('dve', lambda e, o=r16[:], i=X(c): e.tensor_copy(out=o, in_=i), Xcells(c), [('R16', b)])
        self.A(rq16[:], X(c), AF.Square, Xcells(c), [('RQ16', b)])
        self.MM(M[:], self.onesS[:], r16[:], c == 0, c == KC - 1, [('R16', b)], [('PS', bm)], True)
        self.MM(Q[:], self.onesS[:], rq16[:], c == 0, c == KC - 1, [('RQ16', b)], [('PS', bq)], True)

    def ln_finish(self, X, Xcells, gname, bname, outs, banks=(6, 7)):
        p = self.p
        bm, bq = banks
        M, Q = self.PS[bm], self.PS[bq]
        self.A(self.MEAN[:], M[:], AF.Identity, [('PS', bm)], ['MEAN'])
        self.A(self.MSQ[:], M[:], AF.Square, [('PS', bm)], ['MSQ'])
        self.TT('dve', self.MSQ[:], Q[:], self.MSQ[:], ALU.subtract, [('PS', bq), 'MSQ'], ['MSQ'])
        self.A(self.MSQ[:], self.MSQ[:], AF.Ln, ['MSQ'], ['MSQ'], bias=self.epsc[:, 0:1])
        self.A(Q[:], self.MSQ[:], AF.Exp, ['MSQ'], [('PS', bq)], scale=-0.5)
        self.TT('dve', M[:], self.MEAN[:], Q[:], ALU.mult, ['MEAN', ('PS', bq)], [('PS', bm)])
        for c in range(KC):
            self.TT('dve', X(c), X(c), Q[:], ALU.mult, Xcells(c) + [('PS', bq)], Xcells(c))
            self.TT('dve', X(c), X(c), M[:], ALU.subtract, Xcells(c) + [('PS', bm)], Xcells(c))
            for (ofn, cfn, func) in outs:
                self.A(ofn(c), X(c), func, Xcells(c), cfn(c),
                       scale=self.pvc(gname, c), bias=self.pvc(bname, c))

    def layernorm(self, X, Xcells, gname, bname, outs, stats_done=False, banks=(6, 7)):
        if not stats_done:
            for c in range(KC):
                self.ln_stat(X, Xcells, c, banks)
        self.ln_finish(X, Xcells, gname, bname, outs, banks)

    def store_chunk(self, tc):
        ts = slice(tc * TW, (tc + 1) * TW)
        for c in range(KC):
            self.out_toks.append(self.p.dma('sp', self.outT[c * 128:(c + 1) * 128, ts], self.H32[:, c, ts],
                                            reads=[('H32', c, tc)], slot=f"o{c}_{tc}"))
        self.stored.add(tc)

    def load_wpp(self, li):
        self.p.dma('pool', self.WPP[:], self.w_pp[li].rearrange("(k p) n -> p k n", p=128), writes=['WPP'], slot="wpp")

    def ple_stages(self, li, tc):
        p = self.p
        ts = slice(tc * TW, (tc + 1) * TW)
        Hc = lambda c: [('H32', c, tc)]
        H16c = lambda c: [('H16', c, tc)]
        st = {}
        stages = []
        for nh in range(2):
            def ld(nh=nh):
                if nh == 0:
                    pb = st['pb'] = self.alt('pt')
                    self.p.dma('pool', self.PT16[pb][:], self.pT[li][:, ts].rearrange("(k p) t -> p k t", p=128),
                               writes=[('PT16', pb)], slot=f"pt{pb}")
                return self.wload([(self.w_pg[li][:, nh * 512:(nh + 1) * 512].rearrange("(k p) n -> p k n", p=128), KC, 512)])

            def cp(h, nh=nh):
                s, offs, wc = h
                pb = st['pb']
                W = self.wview(s, 0, KC, 512)
                Wp = self.WPP
                for nl in range(4):
                    n = nh * 4 + nl
                    bg = self.alt('psA')
                    GP = self.PS[0 + bg]
                    for kc in range(KC):
                        self.MM(GP[:], W[:, kc, nl * 128:(nl + 1) * 128], self.H16[:, kc, ts], kc == 0, kc == KC - 1,
                                wc + H16c(kc), [('PS', 0 + bg)], kc == KC - 1)
                    bp = self.alt('psB')
                    PP = self.PS[2 + bp]
                    for k2 in range(2):
                        self.MM(PP[:], Wp[:, k2, n * 128:(n + 1) * 128], self.PT16[pb][:, k2, :], k2 == 0, k2 == 1,
                                ['WPP', ('PT16', pb)], [('PS', 2 + bp)], k2 == 1)
                    tb = self.alt('tmp32')
                    tmp = self.TMP32[tb]
                    self.A(tmp[:], GP[:], AF.Sigmoid, [('PS', 0 + bg)], [('TMP32', tb)], bias=self.pvc(f'ple_b{li}', n))
                    self.TT('dve', tmp[:], tmp[:], PP[:], ALU.mult, [('TMP32', tb), ('PS', 2 + bp)], [('TMP32', tb)])
                    self.TT('dve', self.H32[:, n, ts], self.H32[:, n, ts], tmp[:], ALU.add, Hc(n) + [('TMP32', tb)], Hc(n))
                if nh == 1:
                    for n in range(KC):
                        self.A(self.H16[:, n, ts], self.H32[:, n, ts], AF.Copy, Hc(n), H16c(n))
            stages.append((ld, cp))
        return stages

    def layer0_parts(self, tc):
        p = self.p
        ts = slice(tc * TW, (tc + 1) * TW)
        Hc = lambda c: [('H32', c, tc)]
        H16c = lambda c: [('H16', c, tc)]
        U16 = self.U16
        nostage = lambda: None
        P = {}

        def halo():
            if tc == 0:
                p.op('dve', lambda e: e.memset(U16[:, :, 0:CW - 1], 0.0), [], [('U16', c) for c in range(KC)])
            else:
                p.op('dve', lambda e: e.tensor_copy(out=U16[:, :, 0:CW - 1], in_=U16[:, :, TW:TW + CW - 1]),
                     [('U16', c) for c in range(KC)], [('U16', c) for c in range(KC)])
        P['halo'] = halo
        pw1 = []
        for c4 in range(2):
            def ld(c4=c4):
                return self.wload([
                    (self.w_pw1[:, c4 * 512:(c4 + 1) * 512].rearrange("(k p) n -> p k n", p=128), KC, 512)]), \
                    self.wload([
                        (self.w_pw1[:, D + c4 * 512:D + (c4 + 1) * 512].rearrange("(k p) n -> p k n", p=128), KC, 512)])

            def cp(h, c4=c4):
                (sa, _, wca), (sg, _, wcg) = h
                WA = self.wview(sa, 0, KC, 512)
                WG = self.wview(sg, 0, KC, 512)
                for cl in range(4):
                    c = c4 * 4 + cl
                    ba = self.alt('psA')
                    Aps = self.PS[0 + ba]
                    for kc in range(KC):
                        self.MM(Aps[:], WA[:, kc, cl * 128:(cl + 1) * 128], self.H16[:, kc, ts], kc == 0, kc == KC - 1,
                                wca + H16c(kc), [('PS', ba)], kc == KC - 1)
                    bg = self.alt('psB')
                    Gps = self.PS[2 + bg]
                    for kc in range(KC):
                        self.MM(Gps[:], WG[:, kc, cl * 128:(cl + 1) * 128], self.H16[:, kc, ts], kc == 0, kc == KC - 1,
                                wcg + H16c(kc), [('PS', 2 + bg)], kc == KC - 1)
                    tb = self.alt('tmp32')
                    tmp = self.TMP32[tb]
                    self.A(tmp[:], Gps[:], AF.Sigmoid, [('PS', 2 + bg)], [('TMP32', tb)], bias=self.pvc('b_pw1', 8 + c))
                    self.STT(U16[:, c, CW - 1:CW - 1 + TW], Aps[:], self.pvc('b_pw1', c), tmp[:], ALU.add, ALU.mult,
                             [('PS', ba), ('TMP32', tb)], [('U16', c)])
            pw1.append((ld, cp))
        P['pw1'] = pw1
        wo, _ = PV['wdw']

        def conv(c_lo, c_hi):
            for c in range(c_lo, c_hi):
                halves = [list(range(CW - 1, 14, -1)), list(range(14, -1, -1))]
                bc = self.alt('psC')
                Cps = self.PS[4 + bc]
                first = True
                for taps in halves:
                    db = self.alt('dg', 3)
                    Dg = self.DG[db]
                    for i, k in enumerate(taps):
                        col = self.pv[:, wo + c * CW + k: wo + c * CW + k + 1]
                        if i % 2 == 0:
                            p.op('dve', lambda e, o=Dg[:, i, :], col=col: e.tensor_scalar(
                                out=o, in0=self.ident16[:], scalar1=col, scalar2=None, op0=ALU.mult),
                                ['ident16'], [('DG', db, i)])
                        else:
                            self.A(Dg[:, i, :], self.ident16[:], AF.Copy, ['ident16'], [('DG', db, i)], scale=col)
                    for i, k in enumerate(taps):
                        last = (k == 0)
                        self.MM(Cps[:], Dg[:, i, :], U16[:, c, k:k + TW], first, last,
                                [('DG', db, i), ('U16', c)], [('PS', 4 + bc)], last or i == len(taps) - 1)
                        first = False
                self.A(self.V32(c), Cps[:], AF.Identity, [('PS', 4 + bc)], self.V32c(c), bias=self.pvc('b_dw', c))
        P['conv'] = conv
        Y16 = lambda c: self.A16[:, 16 + c, :]
        Y16c = lambda c: [('A16', 16 + c)]
        P['convln'] = lambda: self.layernorm(self.V32, self.V32c, 'cln_g', 'cln_b', [(Y16, Y16c, AF.Silu)])
        HX = lambda c: self.H32[:, c, ts]
        H16X = lambda c: self.H16[:, c, ts]
        pw2 = []
        for nh in range(2):
            def ld(nh=nh):
                return self.wload([(self.w_pw2[:, nh * 512:(nh + 1) * 512].rearrange("(k p) n -> p k n", p=128), KC, 512)])

            def cp(h, nh=nh):
                s, _, wc = h
                W = self.wview(s, 0, KC, 512)
                for nl in range(4):
                    n = nh * 4 + nl
                    ba = self.alt('psA')
                    Mps = self.PS[ba]
                    for c in range(KC):
                        self.MM(Mps[:], W[:, c, nl * 128:(nl + 1) * 128], Y16(c), c == 0, c == KC - 1,
                                wc + Y16c(c), [('PS', ba)], c == KC - 1)
                    tb = self.alt('tmp32')
                    tmp = self.TMP32[tb]
                    self.A(tmp[:], Mps[:], AF.Identity, [('PS', ba)], [('TMP32', tb)], bias=self.pvc('b_pw2', n))
                    self.STT(self.H32[:, n, ts], self.H32[:, n, ts], ALPHA, tmp[:], ALU.mult, ALU.add,
                             Hc(n) + [('TMP32', tb)], Hc(n))
            pw2.append((ld, cp))
        P['pw2'] = pw2
        HX = lambda c: self.H32[:, c, ts]
        H16X = lambda c: self.H16[:, c, ts]
        P['lnm'] = lambda: self.layernorm(HX, Hc, 'lnm_g0', 'lnm_b0', [(H16X, H16c, AF.Identity), (HX, Hc, AF.Identity)])
        gu = []
        for fg in range(6):
            ncol = 512 if fg < 5 else DFF - 5 * 512

            def ld(fg=fg, ncol=ncol):
                return (self.wload([(self.w_fg[:, fg * 512:fg * 512 + ncol].rearrange("(k p) n -> p k n", p=128), KC, ncol)]),
                        self.wload([(self.w_fu[:, fg * 512:fg * 512 + ncol].rearrange("(k p) n -> p k n", p=128), KC, ncol)]))

            def cp(h, fg=fg, ncol=ncol):
                (sg, _, wcg), (su, _, wcu) = h
                WG = self.wview(sg, 0, KC, ncol)
                WU = self.wview(su, 0, KC, ncol)
                for fl in range(ncol // 128):
                    fc = fg * 4 + fl
                    ba = self.alt('psA')
                    Gps = self.PS[ba]
                    for kc in range(KC):
                        self.MM(Gps[:], WG[:, kc, fl * 128:(fl + 1) * 128], self.H16[:, kc, ts], kc == 0, kc == KC - 1,
                                wcg + H16c(kc), [('PS', ba)], kc == KC - 1)
                    bb = self.alt('psB')
                    Ups = self.PS[2 + bb]
                    for kc in range(KC):
                        self.MM(Ups[:], WU[:, kc, fl * 128:(fl + 1) * 128], self.H16[:, kc, ts], kc == 0, kc == KC - 1,
                                wcu + H16c(kc), [('PS', 2 + bb)], kc == KC - 1)
                    sb = self.alt('s16')
                    s16 = self.S16T[sb]
                    self.A(s16[:], Gps[:], AF.Silu, [('PS', ba)], [('S16T', sb)])
                    self.TT('dve', self.A16[:, fc, :], s16[:], Ups[:], ALU.mult, [('S16T', sb), ('PS', 2 + bb)], [('A16', fc)])
            gu.append((ld, cp))
        P['gu'] = gu
        groups = [(0, 8), (8, 8), (16, 6)]
        down = []
        for nh in range(2):
            for gi, (f0, nf) in enumerate(groups):
                def ld(f0=f0, nf=nf, nh=nh):
                    return self.wload([(self.w_fd[f0 * 128:(f0 + nf) * 128, nh * 512:(nh + 1) * 512].rearrange("(k p) n -> p k n", p=128), nf, 512)])

                def cp(h, f0=f0, nf=nf, nh=nh, gi=gi):
                    s, _, wc = h
                    W = self.wview(s, 0, nf, 512)
                    for nl in range(4):
                        Dps = self.PS[4 + nl]
                        for fl in range(nf):
                            fc = f0 + fl
                            self.MM(Dps[:], W[:, fl, nl * 128:(nl + 1) * 128], self.A16[:, fc, :], fc == 0, fc == NFC - 1,
                                    wc + [('A16', fc)], [('PS', 4 + nl)], fl == nf - 1)
                    if gi == len(groups) - 1:
                        for nl in range(4):
                            n = nh * 4 + nl
                            self.STT(self.H32[:, n, ts], self.H32[:, n, ts], ALPHA, self.PS[4 + nl][:], ALU.mult, ALU.add,
                                     Hc(n) + [('PS', 4 + nl)], Hc(n))
                down.append((ld, cp))
        P['down'] = down
        P['lnf'] = lambda: self.layernorm(HX, Hc, 'lnf_g0', 'lnf_b0', [(H16X, H16c, AF.Identity), (HX, Hc, AF.Identity)])
        P['ple'] = self.ple_stages(0, tc)
        return P

    def layer0_all(self):
        nostage = lambda: None
        co = lambda fn: (nostage, (lambda h, fn=fn: fn()))
        parts = [self.layer0_parts(tc) for tc in range(NTC)]
        st = []
        P0 = parts[0]
        st.append(co(P0['halo']))
        st += P0['pw1']
        st.append(co(lambda: P0['conv'](0, KC)))
        for tc in range(NTC):
            P = parts[tc]
            N = parts[tc + 1] if tc + 1 < NTC else None
            if N:
                st.append(co(N['halo']))
                st.append(co(lambda tc=tc: self.cast_x(tc + 1)))
                st.append(N['pw1'][0])
            st.append(co(P['convln']))
            if N:
                st.append(N['pw1'][1])
            st += P['pw2']
            st.append(co(P['lnm']))
            if self.stop == 'h1':
                if N:
                    st.append(co(lambda N=N: N['conv'](0, KC)))
                continue
            st += P['gu']
            st += P['down']
            if self.stop == 'h2':
                st.append(co(P['lnf']))
                if N:
                    st.append(co(lambda N=N: N['conv'](0, KC)))
                continue
            if N:
                st.append(co(lambda N=N: N['conv'](0, 4)))
            st.append(co(P['lnf']))
            if N:
                st.append(co(lambda N=N: N['conv'](4, KC)))
            st.append(P['ple'][0])
            st.append(P['ple'][1])
        self.pipeline(st, lookahead=1)

    def build(self):
        nc = self.nc
        with ExitStack() as st:
            p = self.p = Prog(nc, st)
            sb = lambda name, shape, dt: st.enter_context(nc.sbuf_tensor(name, shape, dt))
            self.H32 = sb("H32", [128, KC, T], F32)
            self.H16 = sb("H16", [128, KC, T], BF16)
            self.WS = [sb(f"WS{i}", [128, 4096], BF16) for i in range(NSLOT)]
            self.pv = sb("pv", [128, NPV], F32)
            self.ident16 = sb("ident16", [128, 128], BF16)
            self.onesS = sb("onesS", [128, 128], BF16)
            self.epsc = sb("epsc", [128, 1], F32)
            self.MEAN = sb("MEAN", [128, TW], F32)
            self.MSQ = sb("MSQ", [128, TW], F32)
            self.TMPALL = sb("TMPALL", [128, 2, TW], F32)
            self.TMP32 = [self.TMPALL[:, i, :] for i in range(2)]
            self.R16 = [sb(f"R16_{i}", [128, TW], BF16) for i in range(2)]
            self.RQ16 = [sb(f"RQ16_{i}", [128, TW], BF16) for i in range(2)]
            self.PSALL = st.enter_context(nc.psum_tensor("PSALL", [128, 8, TW], F32))
            self.PS = [self.PSALL[:, i, :] for i in range(8)]
            p.dma('sp', self.pv[:], self.pvec_d, writes=['pv'], slot='pv')
            p.op('dve', lambda e: e.memset(self.onesS[:], 1.0 / D), [], ['onesS'])
            p.op('dve', lambda e: e.memset(self.epsc[:], EPS), [], ['epsc'])
            p.op('dve', lambda e: e.memset(self.ident16[:], 1.0), [], ['ident16'])
            p.op('pool', lambda e: e.affine_select(out=self.ident16[:], in_=self.ident16[:], pattern=[[1, 128]],
                                                   compare_op=ALU.is_equal, fill=0.0, base=0, channel_multiplier=-1),
                 ['ident16'], ['ident16'])
            def load_x(tc):
                ts = slice(tc * TW, (tc + 1) * TW)
                for c in range(KC):
                    p.dma('sp', self.H32[:, c, ts], self.xT[c * 128:(c + 1) * 128, ts],
                          writes=[('H32', c, tc)], slot=f"x{c}_{tc}")

            def cast_x(tc):
                ts = slice(tc * TW, (tc + 1) * TW)
                for c in range(KC):
                    if c % 2 == 0:
                        self.A(self.H16[:, c, ts], self.H32[:, c, ts], AF.Copy, [('H32', c, tc)], [('H16', c, tc)])
                    else:
                        p.op('dve', lambda e, o=self.H16[:, c, ts], i=self.H32[:, c, ts]: e.tensor_copy(out=o, in_=i),
                             [('H32', c, tc)], [('H16', c, tc)])
            self.cast_x = cast_x
            load_x(0)
            cast_x(0)
            if 0 not in self.layers:
                for tc in range(1, NTC):
                    load_x(tc)
                    cast_x(tc)
            p.barrier()
            if 0 in self.layers:
                for tc in range(1, NTC):
                    load_x(tc)
            if 0 in self.layers:
                with ExitStack() as st0:
                    sb0 = lambda name, shape, dt: st0.enter_context(nc.sbuf_tensor(name, shape, dt))
                    self.U16 = sb0("U16", [128, KC, TW + CW - 1], BF16)
                    self.A16 = sb0("A16", [128, 24, TW], BF16)
                    self.DG = [sb0(f"DG{i}", [128, 16, 128], BF16) for i in range(3)]
                    r16_keep, rq16_keep = self.R16, self.RQ16
                    self.R16 = self.R16 + [sb0(f"R16x_{i}", [128, TW], BF16) for i in range(2)]
                    self.RQ16 = self.RQ16 + [sb0(f"RQ16x_{i}", [128, TW], BF16) for i in range(2)]
                    self.S16T = [sb0(f"S16T_{i}", [128, TW], BF16) for i in range(2)]
                    self.PT16 = [sb0(f"PT16_{i}", [128, 2, TW], BF16) for i in range(2)]
                    self.WPP = sb0("WPP0", [128, 2, D], BF16)
                    self.load_wpp(0)
                    v32 = self.A16[:, 0:16, :].rearrange("p a b -> p (a b)").bitcast(F32).rearrange("p (a b) -> p a b", b=TW)
                    self.V32 = lambda c: v32[:, c, :]
                    self.V32c = lambda c: [('A16', 2 * c), ('A16', 2 * c + 1)]
                    self.layer0_all()
                    p.barrier()
                    self.R16, self.RQ16 = r16_keep, rq16_keep
            if 1 in self.layers:
                from_l1 = True
                self.layer1(st)
            for tc in range(NTC):
                if tc not in self.stored:
                    self.store_chunk(tc)
            p.wait_all('sp', self.out_toks)
            p.emit()
        return nc

    def layer1(self, st):
        p = self.p
        nc = self.nc
        with ExitStack() as sa:
            sb = lambda name, shape, dt: sa.enter_context(nc.sbuf_tensor(name, shape, dt))
            O16 = sb("O16", [128, KC, T], BF16)
            negTri = sb("negTri", [128, 128], BF16)
            negOnes = sb("negOnes", [128, 128], BF16)
            maskneg = sb("maskneg", [128, 128], BF16)
            self.slot_pool = [0, 1]
            QTs = [[sb(f"QT{i}", [128, T], BF16) for i in range(2)],
                   [self.WS[2][:, 0:T], self.WS[2][:, T:2 * T]]]
            KTs = [sb("KT", [128, T], BF16), self.WS[3][:, 0:T]]
            V16s = [sb("V16", [128, 16, 128], BF16), self.WS[3][:, T:2 * T].rearrange("p (a b) -> p a b", b=128)]
            L16 = [sb(f"L16_{i}", [128, 2, TW], BF16) for i in range(2)]
            SS = [sb(f"SS_{i}", [128, 2, TW], BF16) for i in range(2)]
            W16 = [sb(f"W16_{i}", [128, 2, TW], BF16) for i in range(2)]
            E32 = [sb("E32_0", [128, 2, TW], F32), self.TMPALL]
            for st_ in range(2):
                p.op('dve', lambda e, a=QTs[st_][0][64:128, :]: e.memset(a, 0.0), [], [('QTz', st_, 0)])
                p.op('dve', lambda e, a=QTs[st_][1][0:64, :]: e.memset(a, 0.0), [], [('QTz', st_, 1)])
            p.op('dve', lambda e: e.memset(negOnes[:], -1.0), [], ['negOnes'])
            p.op('dve', lambda e: e.memset(maskneg[:], -30000.0), [], ['maskneg'])
            p.op('pool', lambda e: e.affine_select(out=maskneg[:], in_=maskneg[:], pattern=[[-1, 128]],
                                                   compare_op=ALU.is_ge, fill=0.0, base=0, channel_multiplier=1),
                 ['maskneg'], ['maskneg'])
            p.op('dve', lambda e: e.memset(negTri[:], -1.0), [], ['negTri'])
            p.op('pool', lambda e: e.affine_select(out=negTri[:], in_=negTri[:], pattern=[[-1, 128]],
                                                   compare_op=ALU.is_ge, fill=0.0, base=0, channel_multiplier=1),
                 ['negTri'], ['negTri'])
            allH16 = [('H16', c, tc) for c in range(KC) for tc in range(NTC)]

            def load_pair(j):
                return self.wload([(self.w_qkv[:, q * D + j * 128:q * D + (j + 1) * 128].rearrange("(k p) n -> p k n", p=128), KC, 128)
                                   for q in range(3)])

            def proj_items(h, j):
                s_, offs, wc = h
                sx = j % 2
                QT, KT, V16 = QTs[sx], KTs[sx], V16s[sx]
                Wq = self.wview(s_, offs[0], KC, 128)
                Wk = self.wview(s_, offs[1], KC, 128)
                Wv = self.wview(s_, offs[2], KC, 128)
                items = []
                for tc in range(NTC):
                    ts = slice(tc * TW, (tc + 1) * TW)

                    def item_q(tc=tc, ts=ts):
                        ba = self.alt('psA')
                        for kc in range(KC):
                            self.MM(self.PS[ba][:], Wq[:, kc, :], self.H16[:, kc, ts], kc == 0, kc == KC - 1,
                                    wc + [('H16', kc, tc)], [('PS', ba)], kc == KC - 1)
                        p.op('dve', lambda e, o=QT[0][0:64, ts], i=self.PS[ba][0:64, :]: e.tensor_scalar(
                            out=o, in0=i, scalar1=0.125, scalar2=None, op0=ALU.mult), [('PS', ba)], [('QT', sx, tc)])
                        p.op('dve', lambda e, o=QT[1][64:128, ts], i=self.PS[ba][64:128, :]: e.tensor_scalar(
                            out=o, in0=i, scalar1=0.125, scalar2=None, op0=ALU.mult), [('PS', ba)], [('QT', sx, tc)])

                    def item_k(tc=tc, ts=ts):
                        ba = self.alt('psA')
                        for kc in range(KC):
                            self.MM(self.PS[ba][:], Wk[:, kc, :], self.H16[:, kc, ts], kc == 0, kc == KC - 1,
                                    wc + [('H16', kc, tc)], [('PS', ba)], kc == KC - 1)
                        p.op('dve', lambda e, o=KT[:, ts], i=self.PS[ba][:]: e.tensor_copy(out=o, in_=i), [('PS', ba)], [('KT', sx, tc)])

                    vstate = {}

                    def item_v(t4, tc=tc, ts=ts, vstate=vstate):
                        if t4 == 0:
                            vstate['ba'] = self.alt('psA')
                        ba = vstate['ba']
                        tt = tc * 4 + t4
                        for kc in range(KC):
                            self.MM(self.PS[ba][:, t4 * 128:(t4 + 1) * 128], self.H16[:, kc, tt * 128:(tt + 1) * 128], Wv[:, kc, :],
                                    kc == 0, kc == KC - 1, wc + [('H16', kc, tc)], [('PS', ba)], kc == KC - 1)
                        if t4 == 3:
                            p.op('dve', lambda e, o=V16[:, tc * 4:(tc + 1) * 4, :], i=self.PS[ba].rearrange("p (a b) -> p a b", b=128):
                                 e.tensor_copy(out=o, in_=i), [('PS', ba)], [('V16', sx, tc)])
                    items += [item_q, item_k] + [(lambda t4=t4, f=item_v: f(t4)) for t4 in range(4)]
                return items

            def do_pair(j, filler):
                sx = j % 2
                QT, KT, V16 = QTs[sx], KTs[sx], V16s[sx]
                units = []
                for qc in range(NTC):
                    kmax = qc * 4 + 3
                    for kb in range(kmax, -1, -1):
                        units.append(dict(qc=qc, kb=kb, ssb=qc % 2, first=(kb == kmax), last=(kb == 0)))

                def stageA(b):
                    qc, kb = b['qc'], b['kb']
                    i = kb - qc * 4
                    c0 = max(0, 128 * i)
                    cs = slice(c0, TW)
                    qs = slice(qc * TW + c0, (qc + 1) * TW)
                    b.update(i=i, c0=c0, cs=cs)
                    S = SS[b['ssb']]
                    if b['first']:
                        p.op('dve', lambda e, S=S: e.memset(S[:], 0.0), [], [('SS', b['ssb'])])
                    zb = self.alt('psZ', 2)
                    b['zb'] = zb
                    Zd = self.PSALL[:, 2 + 2 * zb:4 + 2 * zb, :]
                    zc = [('PS', 2 + 2 * zb), ('PS', 3 + 2 * zb)]
                    for hh in range(2):
                        self.MM(Zd[:, hh, cs], KT[:, kb * 128:(kb + 1) * 128], QT[hh][:, qs], True, i < 0,
                                [('KT', sx, kb // 4), ('QT', sx, qc), ('QTz', sx, 0), ('QTz', sx, 1)], [zc[hh]], i < 0)
                        if i >= 0:
                            self.MM(Zd[:, hh, c0:c0 + 128], self.ident16[:], maskneg[:], False, True, ['ident16', 'maskneg'],
                                    [zc[hh]], True)
                    eb = self.alt('e32')
                    b['eb'] = eb
                    self.A(E32[eb][:, :, cs], Zd[:, :, cs], AF.Exp, zc, [('E32', eb)])

                def stageA2(b):
                    cs, eb = b['cs'], b['eb']
                    lb = self.alt('l16')
                    b['lb'] = lb
                    self.A(L16[lb][:, :, cs], E32[eb][:, :, cs], AF.Ln, [('E32', eb)], [('L16', lb)], bias=1.0)

                def stageB(b):
                    cs, c0, i = b['cs'], b['c0'], b['i']
                    zb = b['zb']
                    Zd = self.PSALL[:, 2 + 2 * zb:4 + 2 * zb, :]
                    zc = [('PS', 2 + 2 * zb), ('PS', 3 + 2 * zb)]
                    L = L16[b['lb']]
                    lc = ('L16', b['lb'])
                    S = SS[b['ssb']]
                    sc_ = ('SS', b['ssb'])
                    for hh in range(2):
                        self.MM(Zd[:, hh, cs], negTri[:], L[:, hh, cs], False, b['first'], ['negTri', lc], [zc[hh]], True, skip=True)
                        if not b['first']:
                            self.MM(Zd[:, hh, cs], negOnes[:], S[:, hh, cs], False, True, ['negOnes', sc_], [zc[hh]], True, skip=True)
                    if not b['last']:
                        self.TT('dve', S[:, :, cs], S[:, :, cs], L[:, :, cs], ALU.add, [sc_, lc], [sc_])
                    wb = self.alt('w16')
                    b['wb'] = wb
                    self.A(W16[wb][:, :, cs], Zd[:, :, cs], AF.Exp, zc, [('W16', wb)])

                def stageC(b):
                    cs, kb, qc = b['cs'], b['kb'], b['qc']
                    Wt = W16[b['wb']]
                    for hh in range(2):
                        Ob = self.PS[6 + hh]
                        self.MM(Ob[:, cs], V16[:, kb, :], Wt[:, hh, cs], b['first'], b['first'] or b['last'],
                                [('V16', sx, kb // 4), ('W16', b['wb'])], [('PS', 6 + hh)], True, skip=not b['first'])
                        if b['last']:
                            r0 = hh * 64
                            p.op('dve', lambda e, o=O16[r0:r0 + 64, j, qc * TW:(qc + 1) * TW], i=Ob[r0:r0 + 64, :]:
                                 e.tensor_copy(out=o, in_=i), [('PS', 6 + hh)], [('O16', j, qc)])

                nb = len(units)
                for sidx in range(nb + 2):
                    if sidx < nb:
                        stageA(units[sidx])
                    if 0 <= sidx - 1 < nb:
                        stageB(units[sidx - 1])
                    if sidx < nb:
                        stageA2(units[sidx])
                    if 0 <= sidx - 2 < nb:
                        stageC(units[sidx - 2])
                    if filler and sidx % 3 != 0:
                        filler.pop(0)()
                while filler:
                    filler.pop(0)()

            h_cur = load_pair(0)
            for it in proj_items(h_cur, 0):
                it()
            self._release(h_cur)
            for j in range(KC):
                filler = []
                h_next = None
                if j + 1 < KC:
                    h_next = load_pair(j + 1)
                    filler = proj_items(h_next, j + 1)
                do_pair(j, filler)
                self._release(h_next)
            if self.stop == 'o':
                for c in range(KC):
                    for tc in range(NTC):
                        ts = slice(tc * TW, (tc + 1) * TW)
                        self.A(self.H32[:, c, ts], O16[:, c, ts], AF.Copy, [('O16', c, tc)], [('H32', c, tc)])
                p.barrier()
                self.slot_pool = list(range(NSLOT))
                return
            wost = []
            for tc in range(NTC):
                ts = slice(tc * TW, (tc + 1) * TW)
                Hc = lambda c, tc=tc: [('H32', c, tc)]
                H16c = lambda c, tc=tc: [('H16', c, tc)]
                for nh in range(2):
                    def ld(nh=nh):
                        return self.wload([(self.w_o[:, nh * 512:(nh + 1) * 512].rearrange("(k p) n -> p k n", p=128), KC, 512)])

                    def cp(h, nh=nh, tc=tc, ts=ts, Hc=Hc):
                        s, _, wc = h
                        W = self.wview(s, 0, KC, 512)
                        for nl in range(4):
                            n = nh * 4 + nl
                            ba = self.alt('psA')
                            Mps = self.PS[ba]
                            for c in range(KC):
                                self.MM(Mps[:], W[:, c, nl * 128:(nl + 1) * 128], O16[:, c, ts], c == 0, c == KC - 1,
                                        wc + [('O16', c, tc)], [('PS', ba)], c == KC - 1)
                            self.STT(self.H32[:, n, ts], self.H32[:, n, ts], ALPHA, Mps[:], ALU.mult, ALU.add,
                                     Hc(n) + [('PS', ba)], Hc(n))
                    wost.append((ld, cp))

                def lnfn(tc=tc, ts=ts, Hc=Hc, H16c=H16c):
                    HX = lambda c, ts=ts: self.H32[:, c, ts]
                    H16X = lambda c, ts=ts: self.H16[:, c, ts]
                    self.layernorm(HX, Hc, 'lnm_g1', 'lnm_b1', [(H16X, H16c, AF.Identity), (HX, Hc, AF.Identity)],
                                   banks=((6, 7) if tc % 2 == 0 else (4, 5)))
                wost.append(((lambda: None), (lambda h, f=lnfn: f())))
            order = []
            for tc in range(NTC):
                a, b_, l = wost[3 * tc], wost[3 * tc + 1], wost[3 * tc + 2]
                order += [a, b_]
                if tc > 0:
                    order.append(wost[3 * (tc - 1) + 2])
            order.append(wost[3 * (NTC - 1) + 2])
            self.pipeline(order, lookahead=1)
            p.barrier()
            self.slot_pool = list(range(NSLOT))
        if self.stop == 'h1':
            return
        with ExitStack() as sm:
            sb = lambda name, shape, dt: sm.enter_context(nc.sbuf_tensor(name, shape, dt))
            ACTM = [sb(f"ACTM{i}", [128, NFE, TW], BF16) for i in range(2)]
            GATES = [sb(f"GATE16_{i}", [128, NE, TW], BF16) for i in range(2)]
            self.S16T = [sb(f"S16Tm_{i}", [128, TW], BF16) for i in range(2)]
            self.PT16 = [sb(f"PT16m_{i}", [128, 2, TW], BF16) for i in range(2)]
            self.WPP = sb("WPP1", [128, 2, D], BF16)
            self.load_wpp(1)
            SEL = sb("SEL", [8, NE, 128], BF16)
            ident32 = sb("ident32", [128, 128], F32)
            WR32 = sb("WR32", [128, KC, NE], F32)
            BR = sb("BR", [128, NE], F32)
            LG4 = sb("LG", [128, 4, NE], F32)
            MX4 = sb("MX", [128, 4, 8], F32)
            G14 = sb("G1", [128, 4, NE], F32)
            G24 = sb("G2", [128, 4, NE], F32)
            SC4 = sb("SC", [128, 4, 4], F32)
            GTTS = [sb(f"GTT{i}", [8, TW], BF16) for i in range(2)]
            p.dma('sp', WR32[:], self.w_r.rearrange("(k p) e -> p k e", p=128), writes=['WR32'], slot='wr')
            p.dma('sp', BR[:], self.b_r, writes=['BR'], slot='br')
            p.op('dve', lambda e: e.memset(ident32[:], 1.0), [], ['ident32'])
            p.op('pool', lambda e: e.affine_select(out=ident32[:], in_=ident32[:], pattern=[[1, 128]],
                                                   compare_op=ALU.is_equal, fill=0.0, base=0, channel_multiplier=-1),
                 ['ident32'], ['ident32'])
            p.op('dve', lambda e: e.memset(SEL[:], 1.0), [], ['SEL'])
            p.op('pool', lambda e: e.affine_select(out=SEL[:], in_=SEL[:], pattern=[[1, NE], [0, 128]],
                                                   compare_op=ALU.is_equal, fill=0.0, base=0, channel_multiplier=-1),
                 ['SEL'], ['SEL'])
            def router(tc):
                ts = slice(tc * TW, (tc + 1) * TW)
                Hc = lambda c, tc=tc: [('H32', c, tc)]
                lb = self.alt('psB')
                Lps = self.PS[2 + lb]
                for t4 in range(4):
                    tsl = slice(tc * TW + t4 * 128, tc * TW + (t4 + 1) * 128)
                    for kc in range(KC):
                        self.MM(Lps[:, t4 * NE:(t4 + 1) * NE], self.H32[:, kc, tsl], WR32[:, kc, :], kc == 0, kc == KC - 1,
                                Hc(kc) + ['WR32'], [('PS', 2 + lb)], kc == KC - 1)
                for t4 in range(4):
                    LG, MX, G1, G2, SC = LG4[:, t4, :], MX4[:, t4, :], G14[:, t4, :], G24[:, t4, :], SC4[:, t4, :]
                    cl, cm, cg1, cg2, cs_ = ('LG', t4), ('MX', t4), ('G1', t4), ('G2', t4), ('SC', t4)
                    self.TT('dve', LG, Lps[:, t4 * NE:(t4 + 1) * NE], BR[:], ALU.add, [('PS', 2 + lb), 'BR'], [cl])
                    p.op('dve', lambda e, MX=MX, LG=LG: e.max(out=MX, in_=LG), [cl], [cm])
                    self.TT('dve', SC[:, 0:1], MX[:, 1:2], MX[:, 0:1], ALU.subtract, [cm], [cs_])
                    self.A(SC[:, 1:2], SC[:, 0:1], AF.Exp, [cs_], [cs_])
                    p.op('dve', lambda e, SC=SC: e.tensor_scalar(out=SC[:, 2:3], in0=SC[:, 1:2], scalar1=1.0, scalar2=None, op0=ALU.add),
                         [cs_], [cs_])
                    p.op('dve', lambda e, SC=SC: e.reciprocal(out=SC[:, 2:3], in_=SC[:, 2:3]), [cs_], [cs_])
                    self.TT('dve', SC[:, 3:4], SC[:, 1:2], SC[:, 2:3], ALU.mult, [cs_], [cs_])
                    p.op('dve', lambda e, G1=G1, LG=LG, MX=MX, SC=SC: e.tensor_scalar(
                        out=G1, in0=LG, scalar1=MX[:, 0:1], scalar2=SC[:, 2:3], op0=ALU.is_equal, op1=ALU.mult), [cl, cm, cs_], [cg1])
                    p.op('dve', lambda e, G2=G2, LG=LG, MX=MX, SC=SC: e.tensor_scalar(
                        out=G2, in0=LG, scalar1=MX[:, 1:2], scalar2=SC[:, 3:4], op0=ALU.is_equal, op1=ALU.mult), [cl, cm, cs_], [cg2])
                    self.TT('dve', G1, G1, G2, ALU.add, [cg1, cg2], [cg1])

            def router_t(tc):
                gb = self.alt('psA')
                GTps = self.PS[gb]
                for t4 in range(4):
                    p.op('pe', lambda e, o=GTps[0:NE, t4 * 128:(t4 + 1) * 128], i=G14[:, t4, :]: e.transpose(out=o, in_=i, identity=ident32[:]),
                         [('G1', t4), 'ident32'], [('PS', gb)])
                self.A(GTTS[tc % 2][:], GTps[0:NE, :], AF.Copy, [('PS', gb)], [('GTT', tc % 2)])

            def router_b(tc, ci):
                GATE16 = GATES[ci]
                GTT = GTTS[tc % 2]
                for ex in range(NE):
                    bb = self.alt('psB')
                    Bps = self.PS[2 + bb]
                    self.MM(Bps[:], SEL[:, ex, :], GTT[:], True, True, ['SEL', ('GTT', tc % 2)], [('PS', 2 + bb)], True)
                    self.A(GATE16[:, ex, :], Bps[:], AF.Copy, [('PS', 2 + bb)], [(('GATE', ci), ex)])

            def prescale(tc):
                ts = slice(tc * TW, (tc + 1) * TW)
                for n in range(KC):
                    p.op('dve', lambda e, a=self.H32[:, n, ts]: e.tensor_scalar(out=a, in0=a, scalar1=ALPHA, scalar2=None, op0=ALU.mult),
                         [('H32', n, tc)], [('H32', n, tc)])

            def expert_stages(tcs):
                stages = []
                fgroups = [(0, 4), (4, 4), (8, 3)]
                for ex in range(NE):
                    for (f0, nf) in fgroups:
                        def ld(ex=ex, f0=f0, nf=nf):
                            return (self.wload([(self.w_mg[ex][:, f0 * 128:(f0 + nf) * 128].rearrange("(k p) n -> p k n", p=128), KC, nf * 128)]),
                                    self.wload([(self.w_mu[ex][:, f0 * 128:(f0 + nf) * 128].rearrange("(k p) n -> p k n", p=128), KC, nf * 128)]))

                        def cp(h, ex=ex, f0=f0, nf=nf):
                            (sg, _, wcg), (su, _, wcu) = h
                            WG = self.wview(sg, 0, KC, nf * 128)
                            WU = self.wview(su, 0, KC, nf * 128)
                            for ci, tc in enumerate(tcs):
                                ts = slice(tc * TW, (tc + 1) * TW)
                                AM = ACTM[ci]
                                GATE16 = GATES[ci]
                                for fl in range(nf):
                                    fc = f0 + fl
                                    ba = self.alt('psA')
                                    Gps = self.PS[ba]
                                    for kc in range(KC):
                                        self.MM(Gps[:], WG[:, kc, fl * 128:(fl + 1) * 128], self.H16[:, kc, ts], kc == 0, kc == KC - 1,
                                                wcg + [('H16', kc, tc)], [('PS', ba)], kc == KC - 1)
                                    bb = self.alt('psB')
                                    Ups = self.PS[2 + bb]
                                    for kc in range(KC):
                                        self.MM(Ups[:], WU[:, kc, fl * 128:(fl + 1) * 128], self.H16[:, kc, ts], kc == 0, kc == KC - 1,
                                                wcu + [('H16', kc, tc)], [('PS', 2 + bb)], kc == KC - 1)
                                    sb_ = self.alt('s16')
                                    s16 = self.S16T[sb_]
                                    self.A(s16[:], Gps[:], AF.Silu, [('PS', ba)], [('S16T', sb_)])
                                    self.TT('dve', AM[:, fc, :], s16[:], Ups[:], ALU.mult, [('S16T', sb_), ('PS', 2 + bb)], [('ACTM', ci, fc)])
                                    self.TT('dve', AM[:, fc, :], AM[:, fc, :], GATE16[:, ex, :], ALU.mult,
                                            [('ACTM', ci, fc), (('GATE', ci), ex)], [('ACTM', ci, fc)])
                        stages.append((ld, cp))
                    for nh in range(2):
                        def ld(ex=ex, nh=nh):
                            return (self.wload([(self.w_md[ex][0:8 * 128, nh * 512:(nh + 1) * 512].rearrange("(k p) n -> p k n", p=128), 8, 512)]),
                                    self.wload([(self.w_md[ex][8 * 128:NFE * 128, nh * 512:(nh + 1) * 512].rearrange("(k p) n -> p k n", p=128), 3, 512)]))

                        def cp(h, ex=ex, nh=nh):
                            (sa, _, wca), (sb2, _, wcb) = h
                            Wa = self.wview(sa, 0, 8, 512)
                            Wb = self.wview(sb2, 0, 3, 512)
                            for ci, tc in enumerate(tcs):
                                ts = slice(tc * TW, (tc + 1) * TW)
                                AM = ACTM[ci]
                                for nl in range(4):
                                    n = nh * 4 + nl
                                    Dps = self.PS[4 + nl]
                                    for fc in range(NFE):
                                        W, fl, wc = (Wa, fc, wca) if fc < 8 else (Wb, fc - 8, wcb)
                                        self.MM(Dps[:], W[:, fl, nl * 128:(nl + 1) * 128], AM[:, fc, :], fc == 0, fc == NFE - 1,
                                                wc + [('ACTM', ci, fc)], [('PS', 4 + nl)], fc == NFE - 1)
                                    self.TT('dve', self.H32[:, n, ts], self.H32[:, n, ts], Dps[:], ALU.add,
                                            [('H32', n, tc), ('PS', 4 + nl)], [('H32', n, tc)])
                        stages.append((ld, cp))
                return stages

            def tail(tc):
                ts = slice(tc * TW, (tc + 1) * TW)
                Hc = lambda c, tc=tc: [('H32', c, tc)]
                H16c = lambda c, tc=tc: [('H16', c, tc)]
                HX = lambda c, ts=ts: self.H32[:, c, ts]
                H16X = lambda c, ts=ts: self.H16[:, c, ts]
                self.layernorm(HX, Hc, 'lnf_g1', 'lnf_b1', [(H16X, H16c, AF.Identity), (HX, Hc, AF.Identity)])

            nostage = lambda: None
            co = lambda fn: (nostage, (lambda h, fn=fn: fn()))
            GROUPS = [[0, 1], [2, 3]]
            router(0)
            router_t(0)
            router(1)
            router_t(1)
            allst = []
            for gi, tcs in enumerate(GROUPS):
                for ci, tc in enumerate(tcs):
                    allst.append(co(lambda tc=tc, ci=ci: (router_b(tc, ci), prescale(tc))))
                es = expert_stages(tcs)
                extra = []
                if gi > 0:
                    for tc in GROUPS[gi - 1]:
                        pl = self.ple_stages(1, tc) if self.stop != 'h2' else []
                        extra.append(co(lambda tc=tc: tail(tc)))
                        extra.append(None)
                        extra += pl
                        if pl:
                            extra.append(co(lambda tc=tc: self.store_chunk(tc)))
                        extra.append(None)
                if gi + 1 < len(GROUPS):
                    for tc in GROUPS[gi + 1]:
                        extra.append(co(lambda tc=tc: router(tc)))
                        extra.append(None)
                        extra.append(co(lambda tc=tc: router_t(tc)))
                        extra.append(None)
                k = 1
                st_ = [es[0]]
                for x in extra:
                    if x is None:
                        if k < len(es):
                            st_.append(es[k])
                            k += 1
                    else:
                        st_.append(x)
                st_ += es[k:]
                allst += st_
            for tc in GROUPS[-1]:
                allst.append(co(lambda tc=tc: tail(tc)))
                if self.stop != 'h2':
                    allst += self.ple_stages(1, tc)
            self.pipeline(allst, lookahead=1)
            p.barrier()


_CACHE = {}


def _get_nc(layers, stop=None):
    if (layers, stop) not in _CACHE:
        _CACHE[(layers, stop)] = Builder(layers, stop=stop).build()
    return _CACHE[(layers, stop)]


def make_in_maps(inp, layers, hT=None):
    f = lambda a: np.ascontiguousarray(np.asarray(a, np.float32))
    pvec = pack_pvec(inp)
    shared = {"pvec": pvec, "w_pg": f(inp['ple_w_gate']), "w_pp": f(inp['ple_w_proj'])}
    if 0 in layers:
        shared.update(w_pw1=f(inp['conv_w_pw1'][0]), w_pw2=f(inp['conv_w_pw2'][0]),
                      w_fg=f(inp['ffn_w_gate'][0]), w_fu=f(inp['ffn_w_up'][0]), w_fd=f(inp['ffn_w_down'][0]))
    if 1 in layers:
        shared.update(w_qkv=f(inp['attn_w_qkv'][0]), w_o=f(inp['attn_w_o'][0]), w_r=f(inp['moe_w_router'][0]),
                      b_r=f(np.broadcast_to(np.asarray(inp['moe_b_router'][0], np.float32)[None, :], (128, NE))),
                      w_mg=f(inp['moe_w_gate'][0]), w_mu=f(inp['moe_w_up'][0]), w_md=f(inp['moe_w_down'][0]))
    x = np.asarray(inp['x'], np.float32)
    pp = np.asarray(inp['p'], np.float32)
    maps = []
    for b in range(x.shape[0]):
        m = dict(shared)
        m["xT"] = f(x[b].T) if hT is None else f(hT[b])
        m["pT"] = f(pp[:, b].transpose(0, 2, 1))
        maps.append(m)
    return maps


def run_layers(inp, layers, hT=None, cores=None, stop=None):
    nc = _get_nc(tuple(layers), stop)
    maps = make_in_maps(inp, layers, hT)
    if cores is not None:
        maps = [maps[i] for i in cores]
    res = run_bass_kernel_spmd(nc, maps, core_ids=list(range(len(maps))))
    return [r["outT"] for r in res.results]


def kernel(**inputs):
    outs = run_layers(inputs, (0, 1))
    return np.stack([o.T for o in outs], axis=0).astype(np.float32)
```

```python
import numpy as np
from contextlib import ExitStack
import concourse.bass as bass
import concourse.mybir as mybir
from concourse.bass_utils import run_bass_kernel_spmd

F32 = mybir.dt.float32
BF16 = mybir.dt.bfloat16
AF = mybir.ActivationFunctionType
ALU = mybir.AluOpType

EPOCH = 2048
T = 2048
D = 1024
KC = 8
TW = 512
NTC = T // TW
DFF = 2816
NFC = DFF // 128
NE = 8
DFE = 1408
NFE = DFE // 128
DPLE = 256
CW = 31
ALPHA = 4.0 ** 0.25
EPS = 1e-5
NSLOT = 4


class Prog:
    ENG = ('pe', 'act', 'dve', 'pool', 'sp')

    def __init__(self, nc, stack):
        self.nc = nc
        self.stack = stack
        self.stream = {e: [] for e in self.ENG}
        self.last_w = {}
        self.readers = {}
        self.dma_cnt = {}
        self.dma_sem = {}
        self.last_tok = {}

    def _deps(self, reads, writes):
        deps = set()
        for c in reads:
            t = self.last_w.get(c)
            if t is not None:
                deps.add(t)
        for c in writes:
            t = self.last_w.get(c)
            if t is not None:
                deps.add(t)
            r = self.readers.get(c)
            if r:
                deps.update(r.values())
        out = set()
        for t in deps:
            if t[0] == 'd':
                out.add(('d', t[1], 16 * self.dma_cnt[t[1]]))
            else:
                out.add(t)
        return out

    def _commit(self, tok, key, reads, writes):
        for c in reads:
            self.readers.setdefault(c, {})[key] = tok
        for c in writes:
            self.last_w[c] = tok
            self.readers[c] = {}

    def op(self, eng, fn, reads=(), writes=(), inc=True):
        deps = self._deps(reads, writes)
        pos = len(self.stream[eng])
        tok = ('c', eng, pos)
        self.stream[eng].append(dict(kind='op', fn=fn, deps=deps, inc=inc))
        self._commit(tok, eng, reads, writes)
        self.last_tok[eng] = tok
        return tok

    def dma(self, eng, out, in_, reads=(), writes=(), slot=None, **kw):
        deps = self._deps(reads, writes)
        n = self.dma_cnt.get(slot, 0) + 1
        self.dma_cnt[slot] = n
        tok = ('d', slot, 16 * n)
        self.stream[eng].append(dict(kind='dma', out=out, in_=in_, deps=deps, slot=slot, kw=kw))
        self._commit(tok, ('d', slot, n), reads, writes)
        self.last_tok[('d', slot)] = tok
        return tok

    def wait_all(self, eng, toks):
        self.stream[eng].append(dict(kind='wait', deps=set(toks)))

    def barrier(self):
        toks = set(self.last_tok.values())
        for e in self.ENG:
            self.stream[e].append(dict(kind='wait', deps=set(toks)))
        self.last_w = {}
        self.readers = {}

    def emit(self):
        nc = self.nc
        gidx = {}
        for e in self.ENG:
            s = self.stream[e]
            g = 0
            idx = [None] * len(s)
            for i, r in enumerate(s):
                if r['kind'] == 'op' and r['inc']:
                    g += 1
                    r['g'] = g
            nxt = None
            for i in range(len(s) - 1, -1, -1):
                r = s[i]
                if r['kind'] == 'op' and r['inc']:
                    nxt = r['g']
                idx[i] = nxt
            gidx[e] = idx
        sems = {}
        for e in self.ENG:
            tot = max([r.get('g', 0) for r in self.stream[e]] + [0])
            for ep in range((tot + EPOCH - 1) // EPOCH):
                sems[(e, ep)] = self.stack.enter_context(nc.semaphore(f"s_{e}_{ep}"))
        for slot in self.dma_cnt:
            self.dma_sem[slot] = self.stack.enter_context(nc.semaphore(f"d_{slot}"))
        block = self.stack.enter_context(nc.Block())
        engobj = {'pe': block.tensor, 'act': block.scalar, 'dve': block.vector,
                  'pool': block.gpsimd, 'sp': block.sync}

        def make(e):
            def body(eng):
                waited = {}
                for pos, r in enumerate(self.stream[e]):
                    need = {}
                    for t in r['deps']:
                        if t[0] == 'c':
                            _, de, dp = t
                            if de == e and (e == 'pe' or dp >= pos):
                                continue
                            g = gidx[de][dp]
                            assert g is not None, (e, pos, t)
                            k = ('c', de)
                            need[k] = max(need.get(k, 0), g)
                        else:
                            _, slot, val = t
                            k = ('d', slot)
                            need[k] = max(need.get(k, 0), val)
                    for k, v in need.items():
                        if waited.get(k, 0) >= v:
                            continue
                        waited[k] = v
                        if k[0] == 'c':
                            ep = (v - 1) // EPOCH
                            eng.wait_ge(sems[(k[1], ep)], (v - 1) % EPOCH + 1)
                        else:
                            eng.wait_ge(self.dma_sem[k[1]], v)
                    if r['kind'] == 'op':
                        ins = r['fn'](eng)
                        if r['inc']:
                            g = r['g']
                            ins.then_inc(sems[(e, (g - 1) // EPOCH)], 1)
                    elif r['kind'] == 'dma':
                        eng.dma_start(out=r['out'], in_=r['in_'], **r['kw']).then_inc(
                            self.dma_sem[r['slot']], 16)
            return body

        for e in self.ENG:
            if self.stream[e]:
                engobj[e](make(e))


def _col(v):
    v = np.asarray(v, np.float32)
    return v.reshape(-1, 128).T


PV = {}


def _pv_layout():
    off = 0
    for name, w in [('b_pw1', 16), ('b_dw', 8), ('cln_g', 8), ('cln_b', 8), ('b_pw2', 8),
                    ('wdw', 8 * CW),
                    ('lnm_g0', 8), ('lnm_b0', 8), ('lnf_g0', 8), ('lnf_b0', 8), ('ple_b0', 8),
                    ('lnm_g1', 8), ('lnm_b1', 8), ('lnf_g1', 8), ('lnf_b1', 8), ('ple_b1', 8)]:
        PV[name] = (off, w)
        off += w
    return off


NPV = _pv_layout()


def pack_pvec(inp):
    pv = np.zeros((128, NPV), np.float32)

    def put(name, arr):
        o, w = PV[name]
        assert arr.shape == (128, w), (name, arr.shape)
        pv[:, o:o + w] = arr
    put('b_pw1', _col(inp['conv_b_pw1'][0]))
    put('b_dw', _col(inp['conv_b_dw'][0]))
    put('cln_g', _col(inp['conv_ln_g'][0]))
    put('cln_b', _col(inp['conv_ln_b'][0]))
    put('b_pw2', _col(inp['conv_b_pw2'][0]))
    wd = np.asarray(inp['conv_w_dw'][0], np.float32)
    wdw = wd.reshape(CW, 8, 128).transpose(2, 1, 0).reshape(128, 8 * CW)
    put('wdw', wdw)
    for i in range(2):
        put(f'lnm_g{i}', _col(inp['ln_mix_g'][i]))
        put(f'lnm_b{i}', _col(inp['ln_mix_b'][i]))
        put(f'lnf_g{i}', _col(inp['ln_ffn_g'][i]))
        put(f'lnf_b{i}', _col(inp['ln_ffn_b'][i]))
        put(f'ple_b{i}', _col(inp['ple_b_gate'][i]))
    return pv


class Builder:
    def __init__(self, layers=(0, 1), dense_moe=True, stop=None):
        self.layers = layers
        self.stop = stop
        nc = self.nc = bass.Bass("TRN2", target_bir_lowering=False)
        dt = lambda name, shape, kind="ExternalInput": nc.dram_tensor(name, shape, F32, kind=kind).ap()
        self.xT = dt("xT", [D, T])
        self.pT = dt("pT", [2, DPLE, T])
        self.pvec_d = dt("pvec", [128, NPV])
        self.outT = dt("outT", [D, T], kind="ExternalOutput")
        if 0 in layers:
            self.w_pw1 = dt("w_pw1", [D, 2 * D])
            self.w_pw2 = dt("w_pw2", [D, D])
            self.w_fg = dt("w_fg", [D, DFF])
            self.w_fu = dt("w_fu", [D, DFF])
            self.w_fd = dt("w_fd", [DFF, D])
        if 1 in layers:
            self.w_qkv = dt("w_qkv", [D, 3 * D])
            self.w_o = dt("w_o", [D, D])
            self.w_r = dt("w_r", [D, NE])
            self.b_r = dt("b_r", [128, NE])
            self.w_mg = dt("w_mg", [NE, D, DFE])
            self.w_mu = dt("w_mu", [NE, D, DFE])
            self.w_md = dt("w_md", [NE, DFE, D])
        self.w_pg = dt("w_pg", [2, D, D])
        self.w_pp = dt("w_pp", [2, DPLE, D])
        self.cnt = {}
        self.out_toks = []
        self.stored = set()
        self.slot_live = {}
        self.slot_pool = list(range(NSLOT))

    def alt(self, key, n=2):
        v = self.cnt.get(key, 0)
        self.cnt[key] = v + 1
        return v % n

    def A(self, out, in_, func, reads, writes, scale=None, bias=None):
        kw = {}
        if scale is not None:
            kw['scale'] = scale
        if bias is not None:
            kw['bias'] = bias
        self.p.op('act', lambda e: e.activation(out=out, in_=in_, func=func, **kw), reads, writes)

    def TT(self, eng, out, in0, in1, op, reads, writes):
        self.p.op(eng, lambda e: e.tensor_tensor(out=out, in0=in0, in1=in1, op=op), reads, writes)

    def STT(self, out, in0, scalar, in1, op0, op1, reads, writes):
        self.p.op('dve', lambda e: e.scalar_tensor_tensor(out=out, in0=in0, scalar=scalar, in1=in1,
                                                          op0=op0, op1=op1), reads, writes)

    def MM(self, out, lhsT, rhs, start, stop, reads, writes, inc, skip=False):
        self.p.op('pe', lambda e: e.matmul(out, lhsT=lhsT, rhs=rhs, start=start, stop=stop, skip_group_check=skip),
                  reads, writes, inc=inc)

    def pvc(self, name, c):
        o, w = PV[name]
        return self.pv[:, o + c:o + c + 1]

    def wload(self, segs):
        pool_ = self.slot_pool
        s = pool_[self.alt(('wslot', len(pool_)), len(pool_))]
        assert not self.slot_live.get(s, False), f"weight slot {s} overwritten while its handle is live"
        self.slot_live[s] = True
        off = 0
        offs = []
        for i, (ap, a, b) in enumerate(segs):
            dst = self.WS[s][:, off:off + a * b].rearrange("p (a b) -> p a b", b=b)
            self.p.dma('pool', dst, ap, writes=[('WS', s, i)], slot=f"ws{s}")
            offs.append(off)
            off += a * b
        assert off <= 4096
        return s, offs, [('WS', s, i) for i in range(len(segs))]

    def _release(self, h):
        if h is None:
            return
        if isinstance(h, tuple) and len(h) == 3 and isinstance(h[0], int):
            self.slot_live[h[0]] = False
        elif isinstance(h, tuple):
            for x in h:
                self._release(x)

    def wview(self, s, off, a, b):
        return self.WS[s][:, off:off + a * b].rearrange("p (a b) -> p a b", b=b)

    def pipeline(self, stages, lookahead=NSLOT - 1):
        handles = {}
        n = len(stages)
        for i in range(n + lookahead):
            if i < n:
                handles[i] = stages[i][0]()
            j = i - lookahead
            if j >= 0:
                h = handles.pop(j)
                stages[j][1](h)
                self._release(h)

    def ln_stat(self, X, Xcells, c, banks=(6, 7)):
        p = self.p
        bm, bq = banks
        M, Q = self.PS[bm], self.PS[bq]
        b = self.alt(('lnrot', len(self.R16)), len(self.R16))
        r16, rq16 = self.R16[b], self.RQ16[b]
        p.op('dve', lambda e, o=r16[:], i=X(c): e.tensor_copy(out=o, in_=i), Xcells(c), [('R16', b)])
        self.A(rq16[:], X(c), AF.Square, Xcells(c), [('RQ16', b)])
        self.MM(M[:], self.onesS[:], r16[:], c == 0, c == KC - 1, [('R16', b)], [('PS', bm)], True)
        self.MM(Q[:], self.onesS[:], rq16[:], c == 0, c == KC - 1, [('RQ16', b)], [('PS', bq)], True)

    def ln_finish(self, X, Xcells, gname, bname, outs, banks=(6, 7)):
        p = self.p
        bm, bq = banks
        M, Q = self.PS[bm], self.PS[bq]
        self.A(self.MEAN[:], M[:], AF.Identity, [('PS', bm)], ['MEAN'])
        self.A(self.MSQ[:], M[:], AF.Square, [('PS', bm)], ['MSQ'])
        self.TT('dve', self.MSQ[:], Q[:], self.MSQ[:], ALU.subtract, [('PS', bq), 'MSQ'], ['MSQ'])
        self.A(self.MSQ[:], self.MSQ[:], AF.Ln, ['MSQ'], ['MSQ'], bias=self.epsc[:, 0:1])
        self.A(Q[:], self.MSQ[:], AF.Exp, ['MSQ'], [('PS', bq)], scale=-0.5)
        self.TT('dve', M[:], self.MEAN[:], Q[:], ALU.mult, ['MEAN', ('PS', bq)], [('PS', bm)])
        for c in range(KC):
            self.TT('dve', X(c), X(c), Q[:], ALU.mult, Xcells(c) + [('PS', bq)], Xcells(c))
            self.TT('dve', X(c), X(c), M[:], ALU.subtract, Xcells(c) + [('PS', bm)], Xcells(c))
            for (ofn, cfn, func) in outs:
                self.A(ofn(c), X(c), func, Xcells(c), cfn(c),
                       scale=self.pvc(gname, c), bias=self.pvc(bname, c))

    def layernorm(self, X, Xcells, gname, bname, outs, stats_done=False, banks=(6, 7)):
        if not stats_done:
            for c in range(KC):
                self.ln_stat(X, Xcells, c, banks)
        self.ln_finish(X, Xcells, gname, bname, outs, banks)

    def store_chunk(self, tc):
        ts = slice(tc * TW, (tc + 1) * TW)
        for c in range(KC):
            self.out_toks.append(self.p.dma('sp', self.outT[c * 128:(c + 1) * 128, ts], self.H32[:, c, ts],
                                            reads=[('H32', c, tc)], slot=f"o{c}_{tc}"))
        self.stored.add(tc)

    def load_wpp(self, li):
        self.p.dma('pool', self.WPP[:], self.w_pp[li].rearrange("(k p) n -> p k n", p=128), writes=['WPP'], slot="wpp")

    def ple_stages(self, li, tc):
        p = self.p
        ts = slice(tc * TW, (tc + 1) * TW)
        Hc = lambda c: [('H32', c, tc)]
        H16c = lambda c: [('H16', c, tc)]
        st = {}
        stages = []
        for nh in range(2):
            def ld(nh=nh):
                if nh == 0:
                    pb = st['pb'] = self.alt('pt')
                    self.p.dma('pool', self.PT16[pb][:], self.pT[li][:, ts].rearrange("(k p) t -> p k t", p=128),
                               writes=[('PT16', pb)], slot=f"pt{pb}")
                return self.wload([(self.w_pg[li][:, nh * 512:(nh + 1) * 512].rearrange("(k p) n -> p k n", p=128), KC, 512)])

            def cp(h, nh=nh):
                s, offs, wc = h
                pb = st['pb']
                W = self.wview(s, 0, KC, 512)
                Wp = self.WPP
                for nl in range(4):
                    n = nh * 4 + nl
                    bg = self.alt('psA')
                    GP = self.PS[0 + bg]
                    for kc in range(KC):
                        self.MM(GP[:], W[:, kc, nl * 128:(nl + 1) * 128], self.H16[:, kc, ts], kc == 0, kc == KC - 1,
                                wc + H16c(kc), [('PS', 0 + bg)], kc == KC - 1)
                    bp = self.alt('psB')
                    PP = self.PS[2 + bp]
                    for k2 in range(2):
                        self.MM(PP[:], Wp[:, k2, n * 128:(n + 1) * 128], self.PT16[pb][:, k2, :], k2 == 0, k2 == 1,
                                ['WPP', ('PT16', pb)], [('PS', 2 + bp)], k2 == 1)
                    tb = self.alt('tmp32')
                    tmp = self.TMP32[tb]
                    self.A(tmp[:], GP[:], AF.Sigmoid, [('PS', 0 + bg)], [('TMP32', tb)], bias=self.pvc(f'ple_b{li}', n))
                    self.TT('dve', tmp[:], tmp[:], PP[:], ALU.mult, [('TMP32', tb), ('PS', 2 + bp)], [('TMP32', tb)])
                    self.TT('dve', self.H32[:, n, ts], self.H32[:, n, ts], tmp[:], ALU.add, Hc(n) + [('TMP32', tb)], Hc(n))
                if nh == 1:
                    for n in range(KC):
                        self.A(self.H16[:, n, ts], self.H32[:, n, ts], AF.Copy, Hc(n), H16c(n))
            stages.append((ld, cp))
        return stages

    def layer0_parts(self, tc):
        p = self.p
        ts = slice(tc * TW, (tc + 1) * TW)
        Hc = lambda c: [('H32', c, tc)]
        H16c = lambda c: [('H16', c, tc)]
        U16 = self.U16
        nostage = lambda: None
        P = {}

        def halo():
            if tc == 0:
                p.op('dve', lambda e: e.memset(U16[:, :, 0:CW - 1], 0.0), [], [('U16', c) for c in range(KC)])
            else:
                p.op('dve', lambda e: e.tensor_copy(out=U16[:, :, 0:CW - 1], in_=U16[:, :, TW:TW + CW - 1]),
                     [('U16', c) for c in range(KC)], [('U16', c) for c in range(KC)])
        P['halo'] = halo
        pw1 = []
        for c4 in range(2):
            def ld(c4=c4):
                return self.wload([
                    (self.w_pw1[:, c4 * 512:(c4 + 1) * 512].rearrange("(k p) n -> p k n", p=128), KC, 512)]), \
                    self.wload([
                        (self.w_pw1[:, D + c4 * 512:D + (c4 + 1) * 512].rearrange("(k p) n -> p k n", p=128), KC, 512)])

            def cp(h, c4=c4):
                (sa, _, wca), (sg, _, wcg) = h
                WA = self.wview(sa, 0, KC, 512)
                WG = self.wview(sg, 0, KC, 512)
                for cl in range(4):
                    c = c4 * 4 + cl
                    ba = self.alt('psA')
                    Aps = self.PS[0 + ba]
                    for kc in range(KC):
                        self.MM(Aps[:], WA[:, kc, cl * 128:(cl + 1) * 128], self.H16[:, kc, ts], kc == 0, kc == KC - 1,
                                wca + H16c(kc), [('PS', ba)], kc == KC - 1)
                    bg = self.alt('psB')
                    Gps = self.PS[2 + bg]
                    for kc in range(KC):
                        self.MM(Gps[:], WG[:, kc, cl * 128:(cl + 1) * 128], self.H16[:, kc, ts], kc == 0, kc == KC - 1,
                                wcg + H16c(kc), [('PS', 2 + bg)], kc == KC - 1)
                    tb = self.alt('tmp32')
                    tmp = self.TMP32[tb]
                    self.A(tmp[:], Gps[:], AF.Sigmoid, [('PS', 2 + bg)], [('TMP32', tb)], bias=self.pvc('b_pw1', 8 + c))
                    self.STT(U16[:, c, CW - 1:CW - 1 + TW], Aps[:], self.pvc('b_pw1', c), tmp[:], ALU.add, ALU.mult,
                             [('PS', ba), ('TMP32', tb)], [('U16', c)])
            pw1.append((ld, cp))
        P['pw1'] = pw1
        wo, _ = PV['wdw']

        def conv(c_lo, c_hi):
            for c in range(c_lo, c_hi):
                halves = [list(range(CW - 1, 14, -1)), list(range(14, -1, -1))]
                bc = self.alt('psC')
                Cps = self.PS[4 + bc]
                first = True
                for taps in halves:
                    db = self.alt('dg', 3)
                    Dg = self.DG[db]
                    for i, k in enumerate(taps):
                        col = self.pv[:, wo + c * CW + k: wo + c * CW + k + 1]
                        if i % 2 == 0:
                            p.op('dve', lambda e, o=Dg[:, i, :], col=col: e.tensor_scalar(
                                out=o, in0=self.ident16[:], scalar1=col, scalar2=None, op0=ALU.mult),
                                ['ident16'], [('DG', db, i)])
                        else:
                            self.A(Dg[:, i, :], self.ident16[:], AF.Copy, ['ident16'], [('DG', db, i)], scale=col)
                    for i, k in enumerate(taps):
                        last = (k == 0)
                        self.MM(Cps[:], Dg[:, i, :], U16[:, c, k:k + TW], first, last,
                                [('DG', db, i), ('U16', c)], [('PS', 4 + bc)], last or i == len(taps) - 1)
                        first = False
                self.A(self.V32(c), Cps[:], AF.Identity, [('PS', 4 + bc)], self.V32c(c), bias=self.pvc('b_dw', c))
        P['conv'] = conv
        Y16 = lambda c: self.A16[:, 16 + c, :]
        Y16c = lambda c: [('A16', 16 + c)]
        P['convln'] = lambda: self.layernorm(self.V32, self.V32c, 'cln_g', 'cln_b', [(Y16, Y16c, AF.Silu)])
        HX = lambda c: self.H32[:, c, ts]
        H16X = lambda c: self.H16[:, c, ts]
        pw2 = []
        for nh in range(2):
            def ld(nh=nh):
                return self.wload([(self.w_pw2[:, nh * 512:(nh + 1) * 512].rearrange("(k p) n -> p k n", p=128), KC, 512)])

            def cp(h, nh=nh):
                s, _, wc = h
                W = self.wview(s, 0, KC, 512)
                for nl in range(4):
                    n = nh * 4 + nl
                    ba = self.alt('psA')
                    Mps = self.PS[ba]
                    for c in range(KC):
                        self.MM(Mps[:], W[:, c, nl * 128:(nl + 1) * 128], Y16(c), c == 0, c == KC - 1,
                                wc + Y16c(c), [('PS', ba)], c == KC - 1)
                    tb = self.alt('tmp32')
                    tmp = self.TMP32[tb]
                    self.A(tmp[:], Mps[:], AF.Identity, [('PS', ba)], [('TMP32', tb)], bias=self.pvc('b_pw2', n))
                    self.STT(self.H32[:, n, ts], self.H32[:, n, ts], ALPHA, tmp[:], ALU.mult, ALU.add,
                             Hc(n) + [('TMP32', tb)], Hc(n))
            pw2.append((ld, cp))
        P['pw2'] = pw2
        HX = lambda c: self.H32[:, c, ts]
        H16X = lambda c: self.H16[:, c, ts]
        P['lnm'] = lambda: self.layernorm(HX, Hc, 'lnm_g0', 'lnm_b0', [(H16X, H16c, AF.Identity), (HX, Hc, AF.Identity)])
        gu = []
        for fg in range(6):
            ncol = 512 if fg < 5 else DFF - 5 * 512

            def ld(fg=fg, ncol=ncol):
                return (self.wload([(self.w_fg[:, fg * 512:fg * 512 + ncol].rearrange("(k p) n -> p k n", p=128), KC, ncol)]),
                        self.wload([(self.w_fu[:, fg * 512:fg * 512 + ncol].rearrange("(k p) n -> p k n", p=128), KC, ncol)]))

            def cp(h, fg=fg, ncol=ncol):
                (sg, _, wcg), (su, _, wcu) = h
                WG = self.wview(sg, 0, KC, ncol)
                WU = self.wview(su, 0, KC, ncol)
                for fl in range(ncol // 128):
                    fc = fg * 4 + fl
                    ba = self.alt('psA')
                    Gps = self.PS[ba]
                    for kc in range(KC):
                        self.MM(Gps[:], WG[:, kc, fl * 128:(fl + 1) * 128], self.H16[:, kc, ts], kc == 0, kc == KC - 1,
                                wcg + H16c(kc), [('PS', ba)], kc == KC - 1)
                    bb = self.alt('psB')
                    Ups = self.PS[2 + bb]
                    for kc in range(KC):
                        self.MM(Ups[:], WU[:, kc, fl * 128:(fl + 1) * 128], self.H16[:, kc, ts], kc == 0, kc == KC - 1,
                                wcu + H16c(kc), [('PS', 2 + bb)], kc == KC - 1)
                    sb = self.alt('s16')
                    s16 = self.S16T[sb]
                    self.A(s16[:], Gps[:], AF.Silu, [('PS', ba)], [('S16T', sb)])
                    self.TT('dve', self.A16[:, fc, :], s16[:], Ups[:], ALU.mult, [('S16T', sb), ('PS', 2 + bb)], [('A16', fc)])
            gu.append((ld, cp))
        P['gu'] = gu
        groups = [(0, 8), (8, 8), (16, 6)]
        down = []
        for nh in range(2):
            for gi, (f0, nf) in enumerate(groups):
                def ld(f0=f0, nf=nf, nh=nh):
                    return self.wload([(self.w_fd[f0 * 128:(f0 + nf) * 128, nh * 512:(nh + 1) * 512].rearrange("(k p) n -> p k n", p=128), nf, 512)])

                def cp(h, f0=f0, nf=nf, nh=nh, gi=gi):
                    s, _, wc = h
                    W = self.wview(s, 0, nf, 512)
                    for nl in range(4):
                        Dps = self.PS[4 + nl]
                        for fl in range(nf):
                            fc = f0 + fl
                            self.MM(Dps[:], W[:, fl, nl * 128:(nl + 1) * 128], self.A16[:, fc, :], fc == 0, fc == NFC - 1,
                                    wc + [('A16', fc)], [('PS', 4 + nl)], fl == nf - 1)
                    if gi == len(groups) - 1:
                        for nl in range(4):
                            n = nh * 4 + nl
                            self.STT(self.H32[:, n, ts], self.H32[:, n, ts], ALPHA, self.PS[4 + nl][:], ALU.mult, ALU.add,
                                     Hc(n) + [('PS', 4 + nl)], Hc(n))
                down.append((ld, cp))
        P['down'] = down
        P['lnf'] = lambda: self.layernorm(HX, Hc, 'lnf_g0', 'lnf_b0', [(H16X, H16c, AF.Identity), (HX, Hc, AF.Identity)])
        P['ple'] = self.ple_stages(0, tc)
        return P

    def layer0_all(self):
        nostage = lambda: None
        co = lambda fn: (nostage, (lambda h, fn=fn: fn()))
        parts = [self.layer0_parts(tc) for tc in range(NTC)]
        st = []
        P0 = parts[0]
        st.append(co(P0['halo']))
        st += P0['pw1']
        st.append(co(lambda: P0['conv'](0, KC)))
        for tc in range(NTC):
            P = parts[tc]
            N = parts[tc + 1] if tc + 1 < NTC else None
            if N:
                st.append(co(N['halo']))
                st.append(co(lambda tc=tc: self.cast_x(tc + 1)))
                st.append(N['pw1'][0])
            st.append(co(P['convln']))
            if N:
                st.append(N['pw1'][1])
            st += P['pw2']
            st.append(co(P['lnm']))
            if self.stop == 'h1':
                if N:
                    st.append(co(lambda N=N: N['conv'](0, KC)))
                continue
            st += P['gu']
            st += P['down']
            if self.stop == 'h2':
                st.append(co(P['lnf']))
                if N:
                    st.append(co(lambda N=N: N['conv'](0, KC)))
                continue
            if N:
                st.append(co(lambda N=N: N['conv'](0, 4)))
            st.append(co(P['lnf']))
            if N:
                st.append(co(lambda N=N: N['conv'](4, KC)))
            st.append(P['ple'][0])
            st.append(P['ple'][1])
        self.pipeline(st, lookahead=1)

    def build(self):
        nc = self.nc
        with ExitStack() as st:
            p = self.p = Prog(nc, st)
            sb = lambda name, shape, dt: st.enter_context(nc.sbuf_tensor(name, shape, dt))
            self.H32 = sb("H32", [128, KC, T], F32)
            self.H16 = sb("H16", [128, KC, T], BF16)
            self.WS = [sb(f"WS{i}", [128, 4096], BF16) for i in range(NSLOT)]
            self.pv = sb("pv", [128, NPV], F32)
            self.ident16 = sb("ident16", [128, 128], BF16)
            self.onesS = sb("onesS", [128, 128], BF16)
            self.epsc = sb("epsc", [128, 1], F32)
            self.MEAN = sb("MEAN", [128, TW], F32)
            self.MSQ = sb("MSQ", [128, TW], F32)
            self.TMPALL = sb("TMPALL", [128, 2, TW], F32)
            self.TMP32 = [self.TMPALL[:, i, :] for i in range(2)]
            self.R16 = [sb(f"R16_{i}", [128, TW], BF16) for i in range(2)]
            self.RQ16 = [sb(f"RQ16_{i}", [128, TW], BF16) for i in range(2)]
            self.PSALL = st.enter_context(nc.psum_tensor("PSALL", [128, 8, TW], F32))
            self.PS = [self.PSALL[:, i, :] for i in range(8)]
            p.dma('sp', self.pv[:], self.pvec_d, writes=['pv'], slot='pv')
            p.op('dve', lambda e: e.memset(self.onesS[:], 1.0 / D), [], ['onesS'])
            p.op('dve', lambda e: e.memset(self.epsc[:], EPS), [], ['epsc'])
            p.op('dve', lambda e: e.memset(self.ident16[:], 1.0), [], ['ident16'])
            p.op('pool', lambda e: e.affine_select(out=self.ident16[:], in_=self.ident16[:], pattern=[[1, 128]],
                                                   compare_op=ALU.is_equal, fill=0.0, base=0, channel_multiplier=-1),
                 ['ident16'], ['ident16'])
            def load_x(tc):
                ts = slice(tc * TW, (tc + 1) * TW)
                for c in range(KC):
                    p.dma('sp', self.H32[:, c, ts], self.xT[c * 128:(c + 1) * 128, ts],
                          writes=[('H32', c, tc)], slot=f"x{c}_{tc}")

            def cast_x(tc):
                ts = slice(tc * TW, (tc + 1) * TW)
                for c in range(KC):
                    if c % 2 == 0:
                        self.A(self.H16[:, c, ts], self.H32[:, c, ts], AF.Copy, [('H32', c, tc)], [('H16', c, tc)])
                    else:
                        p.op('dve', lambda e, o=self.H16[:, c, ts], i=self.H32[:, c, ts]: e.tensor_copy(out=o, in_=i),
                             [('H32', c, tc)], [('H16', c, tc)])
            self.cast_x = cast_x
            load_x(0)
            cast_x(0)
            if 0 not in self.layers:
                for tc in range(1, NTC):
                    load_x(tc)
                    cast_x(tc)
            p.barrier()
            if 0 in self.layers:
                for tc in range(1, NTC):
                    load_x(tc)
            if 0 in self.layers:
                with ExitStack() as st0:
                    sb0 = lambda name, shape, dt: st0.enter_context(nc.sbuf_tensor(name, shape, dt))
                    self.U16 = sb0("U16", [128, KC, TW + CW - 1], BF16)
                    self.A16 = sb0("A16", [128, 24, TW], BF16)
                    self.DG = [sb0(f"DG{i}", [128, 16, 128], BF16) for i in range(3)]
                    r16_keep, rq16_keep = self.R16, self.RQ16
                    self.R16 = self.R16 + [sb0(f"R16x_{i}", [128, TW], BF16) for i in range(2)]
                    self.RQ16 = self.RQ16 + [sb0(f"RQ16x_{i}", [128, TW], BF16) for i in range(2)]
                    self.S16T = [sb0(f"S16T_{i}", [128, TW], BF16) for i in range(2)]
                    self.PT16 = [sb0(f"PT16_{i}", [128, 2, TW], BF16) for i in range(2)]
                    self.WPP = sb0("WPP0", [128, 2, D], BF16)
                    self.load_wpp(0)
                    v32 = self.A16[:, 0:16, :].rearrange("p a b -> p (a b)").bitcast(F32).rearrange("p (a b) -> p a b", b=TW)
                    self.V32 = lambda c: v32[:, c, :]
                    self.V32c = lambda c: [('A16', 2 * c), ('A16', 2 * c + 1)]
                    self.layer0_all()
                    p.barrier()
                    self.R16, self.RQ16 = r16_keep, rq16_keep
            if 1 in self.layers:
                from_l1 = True
                self.layer1(st)
            for tc in range(NTC):
                if tc not in self.stored:
                    self.store_chunk(tc)
            p.wait_all('sp', self.out_toks)
            p.emit()
        return nc

    def layer1(self, st):
        p = self.p
        nc = self.nc
        with ExitStack() as sa:
            sb = lambda name, shape, dt: sa.enter_context(nc.sbuf_tensor(name, shape, dt))
            O16 = sb("O16", [128, KC, T], BF16)
            negTri = sb("negTri", [128, 128], BF16)
            negOnes = sb("negOnes", [128, 128], BF16)
            maskneg = sb("maskneg", [128, 128], BF16)
            self.slot_pool = [0, 1]
            QTs = [[sb(f"QT{i}", [128, T], BF16) for i in range(2)],
                   [self.WS[2][:, 0:T], self.WS[2][:, T:2 * T]]]
            KTs = [sb("KT", [128, T], BF16), self.WS[3][:, 0:T]]
            V16s = [sb("V16", [128, 16, 128], BF16), self.WS[3][:, T:2 * T].rearrange("p (a b) -> p a b", b=128)]
            L16 = [sb(f"L16_{i}", [128, 2, TW], BF16) for i in range(2)]
            SS = [sb(f"SS_{i}", [128, 2, TW], BF16) for i in range(2)]
            W16 = [sb(f"W16_{i}", [128, 2, TW], BF16) for i in range(2)]
            E32 = [sb("E32_0", [128, 2, TW], F32), self.TMPALL]
            for st_ in range(2):
                p.op('dve', lambda e, a=QTs[st_][0][64:128, :]: e.memset(a, 0.0), [], [('QTz', st_, 0)])
                p.op('dve', lambda e, a=QTs[st_][1][0:64, :]: e.memset(a, 0.0), [], [('QTz', st_, 1)])
            p.op('dve', lambda e: e.memset(negOnes[:], -1.0), [], ['negOnes'])
            p.op('dve', lambda e: e.memset(maskneg[:], -30000.0), [], ['maskneg'])
            p.op('pool', lambda e: e.affine_select(out=maskneg[:], in_=maskneg[:], pattern=[[-1, 128]],
                                                   compare_op=ALU.is_ge, fill=0.0, base=0, channel_multiplier=1),
                 ['maskneg'], ['maskneg'])
            p.op('dve', lambda e: e.memset(negTri[:], -1.0), [], ['negTri'])
            p.op('pool', lambda e: e.affine_select(out=negTri[:], in_=negTri[:], pattern=[[-1, 128]],
                                                   compare_op=ALU.is_ge, fill=0.0, base=0, channel_multiplier=1),
                 ['negTri'], ['negTri'])
            allH16 = [('H16', c, tc) for c in range(KC) for tc in range(NTC)]

            def load_pair(j):
                return self.wload([(self.w_qkv[:, q * D + j * 128:q * D + (j + 1) * 128].rearrange("(k p) n -> p k n", p=128), KC, 128)
                                   for q in range(3)])

            def proj_items(h, j):
                s_, offs, wc = h
                sx = j % 2
                QT, KT, V16 = QTs[sx], KTs[sx], V16s[sx]
                Wq = self.wview(s_, offs[0], KC, 128)
                Wk = self.wview(s_, offs[1], KC, 128)
                Wv = self.wview(s_, offs[2], KC, 128)
                items = []
                for tc in range(NTC):
                    ts = slice(tc * TW, (tc + 1) * TW)

                    def item_q(tc=tc, ts=ts):
                        ba = self.alt('psA')
                        for kc in range(KC):
                            self.MM(self.PS[ba][:], Wq[:, kc, :], self.H16[:, kc, ts], kc == 0, kc == KC - 1,
                                    wc + [('H16', kc, tc)], [('PS', ba)], kc == KC - 1)
                        p.op('dve', lambda e, o=QT[0][0:64, ts], i=self.PS[ba][0:64, :]: e.tensor_scalar(
                            out=o, in0=i, scalar1=0.125, scalar2=None, op0=ALU.mult), [('PS', ba)], [('QT', sx, tc)])
                        p.op('dve', lambda e, o=QT[1][64:128, ts], i=self.PS[ba][64:128, :]: e.tensor_scalar(
                            out=o, in0=i, scalar1=0.125, scalar2=None, op0=ALU.mult), [('PS', ba)], [('QT', sx, tc)])

                    def item_k(tc=tc, ts=ts):
                        ba = self.alt('psA')
                        for kc in range(KC):
                            self.MM(self.PS[ba][:], Wk[:, kc, :], self.H16[:, kc, ts], kc == 0, kc == KC - 1,
                                    wc + [('H16', kc, tc)], [('PS', ba)], kc == KC - 1)
                        p.op('dve', lambda e, o=KT[:, ts], i=self.PS[ba][:]: e.tensor_copy(out=o, in_=i), [('PS', ba)], [('KT', sx, tc)])

                    vstate = {}

                    def item_v(t4, tc=tc, ts=ts, vstate=vstate):
                        if t4 == 0:
                            vstate['ba'] = self.alt('psA')
                        ba = vstate['ba']
                        tt = tc * 4 + t4
                        for kc in range(KC):
                            self.MM(self.PS[ba][:, t4 * 128:(t4 + 1) * 128], self.H16[:, kc, tt * 128:(tt + 1) * 128], Wv[:, kc, :],
                                    kc == 0, kc == KC - 1, wc + [('H16', kc, tc)], [('PS', ba)], kc == KC - 1)
                        if t4 == 3:
                            p.op('dve', lambda e, o=V16[:, tc * 4:(tc + 1) * 4, :], i=self.PS[ba].rearrange("p (a b) -> p a b", b=128):
                                 e.tensor_copy(out=o, in_=i), [('PS', ba)], [('V16', sx, tc)])
                    items += [item_q, item_k] + [(lambda t4=t4, f=item_v: f(t4)) for t4 in range(4)]
                return items

            def do_pair(j, filler):
                sx = j % 2
                QT, KT, V16 = QTs[sx], KTs[sx], V16s[sx]
                units = []
                for qc in range(NTC):
                    kmax = qc * 4 + 3
                    for kb in range(kmax, -1, -1):
                        units.append(dict(qc=qc, kb=kb, ssb=qc % 2, first=(kb == kmax), last=(kb == 0)))

                def stageA(b):
                    qc, kb = b['qc'], b['kb']
                    i = kb - qc * 4
                    c0 = max(0, 128 * i)
                    cs = slice(c0, TW)
                    qs = slice(qc * TW + c0, (qc + 1) * TW)
                    b.update(i=i, c0=c0, cs=cs)
                    S = SS[b['ssb']]
                    if b['first']:
                        p.op('dve', lambda e, S=S: e.memset(S[:], 0.0), [], [('SS', b['ssb'])])
                    zb = self.alt('psZ', 2)
                    b['zb'] = zb
                    Zd = self.PSALL[:, 2 + 2 * zb:4 + 2 * zb, :]
                    zc = [('PS', 2 + 2 * zb), ('PS', 3 + 2 * zb)]
                    for hh in range(2):
                        self.MM(Zd[:, hh, cs], KT[:, kb * 128:(kb + 1) * 128], QT[hh][:, qs], True, i < 0,
                                [('KT', sx, kb // 4), ('QT', sx, qc), ('QTz', sx, 0), ('QTz', sx, 1)], [zc[hh]], i < 0)
                        if i >= 0:
                            self.MM(Zd[:, hh, c0:c0 + 128], self.ident16[:], maskneg[:], False, True, ['ident16', 'maskneg'],
                                    [zc[hh]], True)
                    eb = self.alt('e32')
                    b['eb'] = eb
                    self.A(E32[eb][:, :, cs], Zd[:, :, cs], AF.Exp, zc, [('E32', eb)])

                def stageA2(b):
                    cs, eb = b['cs'], b['eb']
                    lb = self.alt('l16')
                    b['lb'] = lb
                    self.A(L16[lb][:, :, cs], E32[eb][:, :, cs], AF.Ln, [('E32', eb)], [('L16', lb)], bias=1.0)

                def stageB(b):
                    cs, c0, i = b['cs'], b['c0'], b['i']
                    zb = b['zb']
                    Zd = self.PSALL[:, 2 + 2 * zb:4 + 2 * zb, :]
                    zc = [('PS', 2 + 2 * zb), ('PS', 3 + 2 * zb)]
                    L = L16[b['lb']]
                    lc = ('L16', b['lb'])
                    S = SS[b['ssb']]
                    sc_ = ('SS', b['ssb'])
                    for hh in range(2):
                        self.MM(Zd[:, hh, cs], negTri[:], L[:, hh, cs], False, b['first'], ['negTri', lc], [zc[hh]], True, skip=True)
                        if not b['first']:
                            self.MM(Zd[:, hh, cs], negOnes[:], S[:, hh, cs], False, True, ['negOnes', sc_], [zc[hh]], True, skip=True)
                    if not b['last']:
                        self.TT('dve', S[:, :, cs], S[:, :, cs], L[:, :, cs], ALU.add, [sc_, lc], [sc_])
                    wb = self.alt('w16')
                    b['wb'] = wb
                    self.A(W16[wb][:, :, cs], Zd[:, :, cs], AF.Exp, zc, [('W16', wb)])

                def stageC(b):
                    cs, kb, qc = b['cs'], b['kb'], b['qc']
                    Wt = W16[b['wb']]
                    for hh in range(2):
                        Ob = self.PS[6 + hh]
                        self.MM(Ob[:, cs], V16[:, kb, :], Wt[:, hh, cs], b['first'], b['first'] or b['last'],
                                [('V16', sx, kb // 4), ('W16', b['wb'])], [('PS', 6 + hh)], True, skip=not b['first'])
                        if b['last']:
                            r0 = hh * 64
                            p.op('dve', lambda e, o=O16[r0:r0 + 64, j, qc * TW:(qc + 1) * TW], i=Ob[r0:r0 + 64, :]:
                                 e.tensor_copy(out=o, in_=i), [('PS', 6 + hh)], [('O16', j, qc)])

                nb = len(units)
                for sidx in range(nb + 2):
                    if sidx < nb:
                        stageA(units[sidx])
                    if 0 <= sidx - 1 < nb:
                        stageB(units[sidx - 1])
                    if sidx < nb:
                        stageA2(units[sidx])
                    if 0 <= sidx - 2 < nb:
                        stageC(units[sidx - 2])
                    if filler and sidx % 3 != 0:
                        filler.pop(0)()
                while filler:
                    filler.pop(0)()

            h_cur = load_pair(0)
            for it in proj_items(h_cur, 0):
                it()
            self._release(h_cur)
            for j in range(KC):
                filler = []
                h_next = None
                if j + 1 < KC:
                    h_next = load_pair(j + 1)
                    filler = proj_items(h_next, j + 1)
                do_pair(j, filler)
                self._release(h_next)
            if self.stop == 'o':
                for c in range(KC):
                    for tc in range(NTC):
                        ts = slice(tc * TW, (tc + 1) * TW)
                        self.A(self.H32[:, c, ts], O16[:, c, ts], AF.Copy, [('O16', c, tc)], [('H32', c, tc)])
                p.barrier()
                self.slot_pool = list(range(NSLOT))
                return
            wost = []
            for tc in range(NTC):
                ts = slice(tc * TW, (tc + 1) * TW)
                Hc = lambda c, tc=tc: [('H32', c, tc)]
                H16c = lambda c, tc=tc: [('H16', c, tc)]
                for nh in range(2):
                    def ld(nh=nh):
                        return self.wload([(self.w_o[:, nh * 512:(nh + 1) * 512].rearrange("(k p) n -> p k n", p=128), KC, 512)])

                    def cp(h, nh=nh, tc=tc, ts=ts, Hc=Hc):
                        s, _, wc = h
                        W = self.wview(s, 0, KC, 512)
                        for nl in range(4):
                            n = nh * 4 + nl
                            ba = self.alt('psA')
                            Mps = self.PS[ba]
                            for c in range(KC):
                                self.MM(Mps[:], W[:, c, nl * 128:(nl + 1) * 128], O16[:, c, ts], c == 0, c == KC - 1,
                                        wc + [('O16', c, tc)], [('PS', ba)], c == KC - 1)
                            self.STT(self.H32[:, n, ts], self.H32[:, n, ts], ALPHA, Mps[:], ALU.mult, ALU.add,
                                     Hc(n) + [('PS', ba)], Hc(n))
                    wost.append((ld, cp))

                def lnfn(tc=tc, ts=ts, Hc=Hc, H16c=H16c):
                    HX = lambda c, ts=ts: self.H32[:, c, ts]
                    H16X = lambda c, ts=ts: self.H16[:, c, ts]
                    self.layernorm(HX, Hc, 'lnm_g1', 'lnm_b1', [(H16X, H16c, AF.Identity), (HX, Hc, AF.Identity)],
                                   banks=((6, 7) if tc % 2 == 0 else (4, 5)))
                wost.append(((lambda: None), (lambda h, f=lnfn: f())))
            order = []
            for tc in range(NTC):
                a, b_, l = wost[3 * tc], wost[3 * tc + 1], wost[3 * tc + 2]
                order += [a, b_]
                if tc > 0:
                    order.append(wost[3 * (tc - 1) + 2])
            order.append(wost[3 * (NTC - 1) + 2])
            self.pipeline(order, lookahead=1)
            p.barrier()
            self.slot_pool = list(range(NSLOT))
        if self.stop == 'h1':
            return
        with ExitStack() as sm:
            sb = lambda name, shape, dt: sm.enter_context(nc.sbuf_tensor(name, shape, dt))
            ACTM = [sb(f"ACTM{i}", [128, NFE, TW], BF16) for i in range(2)]
            GATES = [sb(f"GATE16_{i}", [128, NE, TW], BF16) for i in range(2)]
            self.S16T = [sb(f"S16Tm_{i}", [128, TW], BF16) for i in range(2)]
            self.PT16 = [sb(f"PT16m_{i}", [128, 2, TW], BF16) for i in range(2)]
            self.WPP = sb("WPP1", [128, 2, D], BF16)
            self.load_wpp(1)
            SEL = sb("SEL", [8, NE, 128], BF16)
            ident32 = sb("ident32", [128, 128], F32)
            WR32 = sb("WR32", [128, KC, NE], F32)
            BR = sb("BR", [128, NE], F32)
            LG4 = sb("LG", [128, 4, NE], F32)
            MX4 = sb("MX", [128, 4, 8], F32)
            G14 = sb("G1", [128, 4, NE], F32)
            G24 = sb("G2", [128, 4, NE], F32)
            SC4 = sb("SC", [128, 4, 4], F32)
            GTTS = [sb(f"GTT{i}", [8, TW], BF16) for i in range(2)]
            p.dma('sp', WR32[:], self.w_r.rearrange("(k p) e -> p k e", p=128), writes=['WR32'], slot='wr')
            p.dma('sp', BR[:], self.b_r, writes=['BR'], slot='br')
            p.op('dve', lambda e: e.memset(ident32[:], 1.0), [], ['ident32'])
            p.op('pool', lambda e: e.affine_select(out=ident32[:], in_=ident32[:], pattern=[[1, 128]],
                                                   compare_op=ALU.is_equal, fill=0.0, base=0, channel_multiplier=-1),
                 ['ident32'], ['ident32'])
            p.op('dve', lambda e: e.memset(SEL[:], 1.0), [], ['SEL'])
            p.op('pool', lambda e: e.affine_select(out=SEL[:], in_=SEL[:], pattern=[[1, NE], [0, 128]],
                                                   compare_op=ALU.is_equal, fill=0.0, base=0, channel_multiplier=-1),
                 ['SEL'], ['SEL'])
            def router(tc):
                ts = slice(tc * TW, (tc + 1) * TW)
                Hc = lambda c, tc=tc: [('H32', c, tc)]
                lb = self.alt('psB')
                Lps = self.PS[2 + lb]
                for t4 in range(4):
                    tsl = slice(tc * TW + t4 * 128, tc * TW + (t4 + 1) * 128)
                    for kc in range(KC):
                        self.MM(Lps[:, t4 * NE:(t4 + 1) * NE], self.H32[:, kc, tsl], WR32[:, kc, :], kc == 0, kc == KC - 1,
                                Hc(kc) + ['WR32'], [('PS', 2 + lb)], kc == KC - 1)
                for t4 in range(4):
                    LG, MX, G1, G2, SC = LG4[:, t4, :], MX4[:, t4, :], G14[:, t4, :], G24[:, t4, :], SC4[:, t4, :]
                    cl, cm, cg1, cg2, cs_ = ('LG', t4), ('MX', t4), ('G1', t4), ('G2', t4), ('SC', t4)
                    self.TT('dve', LG, Lps[:, t4 * NE:(t4 + 1) * NE], BR[:], ALU.add, [('PS', 2 + lb), 'BR'], [cl])
                    p.op('dve', lambda e, MX=MX, LG=LG: e.max(out=MX, in_=LG), [cl], [cm])
                    self.TT('dve', SC[:, 0:1], MX[:, 1:2], MX[:, 0:1], ALU.subtract, [cm], [cs_])
                    self.A(SC[:, 1:2], SC[:, 0:1], AF.Exp, [cs_], [cs_])
                    p.op('dve', lambda e, SC=SC: e.tensor_scalar(out=SC[:, 2:3], in0=SC[:, 1:2], scalar1=1.0, scalar2=None, op0=ALU.add),
                         [cs_], [cs_])
                    p.op('dve', lambda e, SC=SC: e.reciprocal(out=SC[:, 2:3], in_=SC[:, 2:3]), [cs_], [cs_])
                    self.TT('dve', SC[:, 3:4], SC[:, 1:2], SC[:, 2:3], ALU.mult, [cs_], [cs_])
                    p.op('dve', lambda e, G1=G1, LG=LG, MX=MX, SC=SC: e.tensor_scalar(
                        out=G1, in0=LG, scalar1=MX[:, 0:1], scalar2=SC[:, 2:3], op0=ALU.is_equal, op1=ALU.mult), [cl, cm, cs_], [cg1])
                    p.op('dve', lambda e, G2=G2, LG=LG, MX=MX, SC=SC: e.tensor_scalar(
                        out=G2, in0=LG, scalar1=MX[:, 1:2], scalar2=SC[:, 3:4], op0=ALU.is_equal, op1=ALU.mult), [cl, cm, cs_], [cg2])
                    self.TT('dve', G1, G1, G2, ALU.add, [cg1, cg2], [cg1])

            def router_t(tc):
                gb = self.alt('psA')
                GTps = self.PS[gb]
                for t4 in range(4):
                    p.op('pe', lambda e, o=GTps[0:NE, t4 * 128:(t4 + 1) * 128], i=G14[:, t4, :]: e.transpose(out=o, in_=i, identity=ident32[:]),
                         [('G1', t4), 'ident32'], [('PS', gb)])
                self.A(GTTS[tc % 2][:], GTps[0:NE, :], AF.Copy, [('PS', gb)], [('GTT', tc % 2)])

            def router_b(tc, ci):
                GATE16 = GATES[ci]
                GTT = GTTS[tc % 2]
                for ex in range(NE):
                    bb = self.alt('psB')
                    Bps = self.PS[2 + bb]
                    self.MM(Bps[:], SEL[:, ex, :], GTT[:], True, True, ['SEL', ('GTT', tc % 2)], [('PS', 2 + bb)], True)
                    self.A(GATE16[:, ex, :], Bps[:], AF.Copy, [('PS', 2 + bb)], [(('GATE', ci), ex)])

            def prescale(tc):
                ts = slice(tc * TW, (tc + 1) * TW)
                for n in range(KC):
                    p.op('dve', lambda e, a=self.H32[:, n, ts]: e.tensor_scalar(out=a, in0=a, scalar1=ALPHA, scalar2=None, op0=ALU.mult),
                         [('H32', n, tc)], [('H32', n, tc)])

            def expert_stages(tcs):
                stages = []
                fgroups = [(0, 4), (4, 4), (8, 3)]
                for ex in range(NE):
                    for (f0, nf) in fgroups:
                        def ld(ex=ex, f0=f0, nf=nf):
                            return (self.wload([(self.w_mg[ex][:, f0 * 128:(f0 + nf) * 128].rearrange("(k p) n -> p k n", p=128), KC, nf * 128)]),
                                    self.wload([(self.w_mu[ex][:, f0 * 128:(f0 + nf) * 128].rearrange("(k p) n -> p k n", p=128), KC, nf * 128)]))

                        def cp(h, ex=ex, f0=f0, nf=nf):
                            (sg, _, wcg), (su, _, wcu) = h
                            WG = self.wview(sg, 0, KC, nf * 128)
                            WU = self.wview(su, 0, KC, nf * 128)
                            for ci, tc in enumerate(tcs):
                                ts = slice(tc * TW, (tc + 1) * TW)
                                AM = ACTM[ci]
                                GATE16 = GATES[ci]
                                for fl in range(nf):
                                    fc = f0 + fl
                                    ba = self.alt('psA')
                                    Gps = self.PS[ba]
                                    for kc in range(KC):
                                        self.MM(Gps[:], WG[:, kc, fl * 128:(fl + 1) * 128], self.H16[:, kc, ts], kc == 0, kc == KC - 1,
                                                wcg + [('H16', kc, tc)], [('PS', ba)], kc == KC - 1)
                                    bb = self.alt('psB')
                                    Ups = self.PS[2 + bb]
                                    for kc in range(KC):
                                        self.MM(Ups[:], WU[:, kc, fl * 128:(fl + 1) * 128], self.H16[:, kc, ts], kc == 0, kc == KC - 1,
                                                wcu + [('H16', kc, tc)], [('PS', 2 + bb)], kc == KC - 1)
                                    sb_ = self.alt('s16')
                                    s16 = self.S16T[sb_]
                                    self.A(s16[:], Gps[:], AF.Silu, [('PS', ba)], [('S16T', sb_)])
                                    self.TT('dve', AM[:, fc, :], s16[:], Ups[:], ALU.mult, [('S16T', sb_), ('PS', 2 + bb)], [('ACTM', ci, fc)])
                                    self.TT('dve', AM[:, fc, :], AM[:, fc, :], GATE16[:, ex, :], ALU.mult,
                                            [('ACTM', ci, fc), (('GATE', ci), ex)], [('ACTM', ci, fc)])
                        stages.append((ld, cp))
                    for nh in range(2):
                        def ld(ex=ex, nh=nh):
                            return (self.wload([(self.w_md[ex][0:8 * 128, nh * 512:(nh + 1) * 512].rearrange("(k p) n -> p k n", p=128), 8, 512)]),
                                    self.wload([(self.w_md[ex][8 * 128:NFE * 128, nh * 512:(nh + 1) * 512].rearrange("(k p) n -> p k n", p=128), 3, 512)]))

                        def cp(h, ex=ex, nh=nh):
                            (sa, _, wca), (sb2, _, wcb) = h
                            Wa = self.wview(sa, 0, 8, 512)
                            Wb = self.wview(sb2, 0, 3, 512)
                            for ci, tc in enumerate(tcs):
                                ts = slice(tc * TW, (tc + 1) * TW)
                                AM = ACTM[ci]
                                for nl in range(4):
                                    n = nh * 4 + nl
                                    Dps = self.PS[4 + nl]
                                    for fc in range(NFE):
                                        W, fl, wc = (Wa, fc, wca) if fc < 8 else (Wb, fc - 8, wcb)
                                        self.MM(Dps[:], W[:, fl, nl * 128:(nl + 1) * 128], AM[:, fc, :], fc == 0, fc == NFE - 1,
                                                wc + [('ACTM', ci, fc)], [('PS', 4 + nl)], fc == NFE - 1)
                                    self.TT('dve', self.H32[:, n, ts], self.H32[:, n, ts], Dps[:], ALU.add,
                                            [('H32', n, tc), ('PS', 4 + nl)], [('H32', n, tc)])
                        stages.append((ld, cp))
                return stages

            def tail(tc):
                ts = slice(tc * TW, (tc + 1) * TW)
                Hc = lambda c, tc=tc: [('H32', c, tc)]
                H16c = lambda c, tc=tc: [('H16', c, tc)]
                HX = lambda c, ts=ts: self.H32[:, c, ts]
                H16X = lambda c, ts=ts: self.H16[:, c, ts]
                self.layernorm(HX, Hc, 'lnf_g1', 'lnf_b1', [(H16X, H16c, AF.Identity), (HX, Hc, AF.Identity)])

            nostage = lambda: None
            co = lambda fn: (nostage, (lambda h, fn=fn: fn()))
            GROUPS = [[0, 1], [2, 3]]
            router(0)
            router_t(0)
            router(1)
            router_t(1)
            allst = []
            for gi, tcs in enumerate(GROUPS):
                for ci, tc in enumerate(tcs):
                    allst.append(co(lambda tc=tc, ci=ci: (router_b(tc, ci), prescale(tc))))
                es = expert_stages(tcs)
                ins = {}
                def put(after, item):
                    ins.setdefault(after, []).append(item)
                slot_ids = [0, 1, 5, 6, 10, 11, 15, 16, 20, 21, 25, 26, 30, 31]
                k = 0
                if gi > 0:
                    for tc in GROUPS[gi - 1]:
                        put(slot_ids[k], co(lambda tc=tc: tail(tc)))
                        k += 1
                        if self.stop != 'h2':
                            pl = self.ple_stages(1, tc)
                            put(slot_ids[k], pl[0])
                            put(slot_ids[k], pl[1])
                            put(slot_ids[k], co(lambda tc=tc: self.store_chunk(tc)))
                        k += 1
                if gi + 1 < len(GROUPS):
                    for tc in GROUPS[gi + 1]:
                        put(slot_ids[k], co(lambda tc=tc: router(tc)))
                        k += 1
                        put(slot_ids[k], co(lambda tc=tc: router_t(tc)))
                        k += 1
                st_ = []
                for idx, e_ in enumerate(es):
                    st_.append(e_)
                    st_ += ins.get(idx, [])
                allst += st_
            for tc in GROUPS[-1]:
                allst.append(co(lambda tc=tc: tail(tc)))
            if self.stop != 'h2':
                for tc in GROUPS[-1]:
                    allst += self.ple_stages(1, tc)
                    allst.append(co(lambda tc=tc: self.store_chunk(tc)))
            self.pipeline(allst, lookahead=1)
            p.barrier()


_CACHE = {}


def _get_nc(layers, stop=None):
    if (layers, stop) not in _CACHE:
        _CACHE[(layers, stop)] = Builder(layers, stop=stop).build()
    return _CACHE[(layers, stop)]


def make_in_maps(inp, layers, hT=None):
    f = lambda a: np.ascontiguousarray(np.asarray(a, np.float32))
    pvec = pack_pvec(inp)
    shared = {"pvec": pvec, "w_pg": f(inp['ple_w_gate']), "w_pp": f(inp['ple_w_proj'])}
    if 0 in layers:
        shared.update(w_pw1=f(inp['conv_w_pw1'][0]), w_pw2=f(inp['conv_w_pw2'][0]),
                      w_fg=f(inp['ffn_w_gate'][0]), w_fu=f(inp['ffn_w_up'][0]), w_fd=f(inp['ffn_w_down'][0]))
    if 1 in layers:
        shared.update(w_qkv=f(inp['attn_w_qkv'][0]), w_o=f(inp['attn_w_o'][0]), w_r=f(inp['moe_w_router'][0]),
                      b_r=f(np.broadcast_to(np.asarray(inp['moe_b_router'][0], np.float32)[None, :], (128, NE))),
                      w_mg=f(inp['moe_w_gate'][0]), w_mu=f(inp['moe_w_up'][0]), w_md=f(inp['moe_w_down'][0]))
    x = np.asarray(inp['x'], np.float32)
    pp = np.asarray(inp['p'], np.float32)
    maps = []
    for b in range(x.shape[0]):
        m = dict(shared)
        m["xT"] = f(x[b].T) if hT is None else f(hT[b])
        m["pT"] = f(pp[:, b].transpose(0, 2, 1))
        maps.append(m)
    return maps


def run_layers(inp, layers, hT=None, cores=None, stop=None):
    nc = _get_nc(tuple(layers), stop)
    maps = make_in_maps(inp, layers, hT)
    if cores is not None:
        maps = [maps[i] for i in cores]
    res = run_bass_kernel_spmd(nc, maps, core_ids=list(range(len(maps))))
    return [r["outT"] for r in res.results]


def kernel(**inputs):
    outs = run_layers(inputs, (0, 1))
    return np.stack([o.T for o in outs], axis=0).astype(np.float32)
```
